# Optimizing a Trainium2 kernel written in Bass

```python
import math
import jax, jax.numpy as jnp
from jax import lax
import numpy as np

D_MODEL = 1024
BATCH = 2
SEQ = 16384
DEPTH = 4

MIX_WIDTH = D_MODEL
S5_WIDTH = MIX_WIDTH // 2
POOL_WIDTH = MIX_WIDTH - S5_WIDTH
S5_GROUP = 16
S5_GROUPS = S5_WIDTH // S5_GROUP
S5_STATE = 64
DT_MIN = 0.001
DT_MAX = 0.1
POOL_WINDOWS = (2, 4, 8, 16)
POOL_GROUP = POOL_WIDTH // len(POOL_WINDOWS)
ATT_HEAD_DIM = 64
ATT_HEADS = D_MODEL // (2 * ATT_HEAD_DIM)
ATT_V_DIM = 2 * ATT_HEAD_DIM
QK_WIDTH = ATT_HEADS * 2 * ATT_HEAD_DIM
Q_BLOCK = 128
ROPE_THETA = 10000.0
N_EXPERTS = 16
EXPERT_FF = 2 * D_MODEL
CAPACITY_FACTOR = 2
EPS = 1e-6
N_EVEN = (DEPTH + 1) // 2
N_ODD = DEPTH // 2

kernel_name = 'hybrid_s5_pool_diffattn_ec_moe'

F32 = jnp.float32


def rmsnorm(x, g):
    xf = x.astype(F32)
    y = xf * lax.rsqrt(jnp.mean(xf * xf, axis=-1, keepdims=True) + EPS)
    return (y * g.astype(F32)).astype(x.dtype)


def rope_tables(positions, dim):
    inv = ROPE_THETA ** (-jnp.arange(0, dim, 2, dtype=F32) / dim)
    ang = positions.astype(F32)[..., None] * inv
    return jnp.cos(ang), jnp.sin(ang)


def apply_rope(x, cos, sin):
    x1, x2 = jnp.split(x, 2, axis=-1)
    c = cos[:, :, None, None, :]
    s = sin[:, :, None, None, :]
    return jnp.concatenate([x1 * c - x2 * s, x1 * s + x2 * c], axis=-1).astype(x.dtype)


def _ssm_combine(left, right):
    a_l, b_l = left
    a_r, b_r = right
    return a_r * a_l, a_r * b_l + b_r


def s5_direction(u, lam_re, lam_im, log_dt, b_re, b_im, c_re, c_im, reverse):
    lam = lax.complex(lam_re.astype(F32), lam_im.astype(F32))
    dt = jnp.exp(log_dt.astype(F32))[:, None]
    a_bar = jnp.exp(lam * dt)
    b = lax.complex(b_re.astype(F32), b_im.astype(F32))
    b_bar = ((a_bar - 1.0) / lam)[..., None] * b
    bu = jnp.einsum('bsgc,gpc->bsgp', u.astype(jnp.complex64), b_bar)
    a = jnp.broadcast_to(a_bar, bu.shape)
    _, h = lax.associative_scan(_ssm_combine, (a, bu), reverse=reverse, axis=1)
    c = lax.complex(c_re.astype(F32), c_im.astype(F32))
    return jnp.einsum('bsgp,gcp->bsgc', h, c).real


def s5_mixer(u, lam_re, lam_im, log_dt, b_re, b_im, c_re, c_im, d_skip, w_glu, b_glu):
    bsz, seq, _ = u.shape
    uf = u.astype(F32)
    ug = uf.reshape(bsz, seq, S5_GROUPS, S5_GROUP)
    y = d_skip.astype(F32) * uf
    for direction, rev in ((0, False), (1, True)):
        y = y + s5_direction(ug, lam_re[direction], lam_im[direction], log_dt[direction],
                             b_re[direction], b_im[direction], c_re[direction], c_im[direction],
                             rev).reshape(bsz, seq, S5_WIDTH)
    y = jax.nn.gelu(y)
    y = y * jax.nn.sigmoid(y @ w_glu.astype(F32) + b_glu.astype(F32))
    return y.astype(u.dtype)


def pool_mixer(v, w_group, scale):
    bsz, seq, _ = v.shape
    vf = v.astype(F32)
    cs = jnp.concatenate([jnp.zeros((bsz, 1, POOL_WIDTH), F32), lax.cumsum(vf, axis=1)], axis=1)
    t = jnp.arange(seq)
    outs = []
    for gi, win in enumerate(POOL_WINDOWS):
        lo = jnp.clip(t - win // 2, 0, seq)
        hi = jnp.clip(t + win // 2, 0, seq)
        sl = slice(gi * POOL_GROUP, (gi + 1) * POOL_GROUP)
        csg = cs[:, :, sl]
        cnt = (hi - lo).astype(F32)[None, :, None]
        outs.append((csg[:, hi] - csg[:, lo]) / cnt - vf[:, :, sl])
    pooled = jnp.stack(outs, axis=2)
    y = jnp.einsum('bsgc,gcd->bsgd', pooled, w_group.astype(F32)).reshape(bsz, seq, POOL_WIDTH)
    return (y * scale.astype(F32)).astype(v.dtype)


def diff_attention(h, cos, sin, w_qkv, w_out, q_norm_g, k_norm_g, lam_q1, lam_k1, lam_q2, lam_k2,
                   subln_g, layer_idx):
    bsz, seq, _ = h.shape
    nb = seq // Q_BLOCK
    qkv = h @ w_qkv
    q, k, v = jnp.split(qkv, [QK_WIDTH, 2 * QK_WIDTH], axis=-1)
    q = q.reshape(bsz, seq, ATT_HEADS, 2, ATT_HEAD_DIM)
    k = k.reshape(bsz, seq, ATT_HEADS, 2, ATT_HEAD_DIM)
    v = v.reshape(bsz, seq, ATT_HEADS, ATT_V_DIM)
    q = apply_rope(rmsnorm(q, q_norm_g), cos, sin)
    k = apply_rope(rmsnorm(k, k_norm_g), cos, sin)
    lam_init = 0.8 - 0.6 * math.exp(-0.3 * layer_idx)
    lam = (jnp.exp(jnp.sum(lam_q1.astype(F32) * lam_k1.astype(F32)))
           - jnp.exp(jnp.sum(lam_q2.astype(F32) * lam_k2.astype(F32))) + lam_init)
    qb = (q * (ATT_HEAD_DIM ** -0.5)).reshape(bsz, nb, Q_BLOCK, ATT_HEADS, 2, ATT_HEAD_DIM)
    qb = qb.transpose(1, 0, 3, 4, 2, 5)
    kt = k.transpose(0, 2, 3, 1, 4)
    vt = v.transpose(0, 2, 1, 3)

    def block(qblk):
        s = jnp.einsum('bhtqd,bhtkd->bhtqk', qblk, kt).astype(F32)
        p = jax.nn.softmax(s, axis=-1)
        p = p[:, :, 0] - lam * p[:, :, 1]
        return jnp.einsum('bhqk,bhkd->bhqd', p.astype(vt.dtype), vt)

    o = lax.map(block, qb)
    o = o.transpose(1, 0, 3, 2, 4).reshape(bsz, seq, ATT_HEADS, ATT_V_DIM)
    o = rmsnorm(o, subln_g) * (1.0 - lam_init)
    return o.reshape(bsz, seq, ATT_HEADS * ATT_V_DIM) @ w_out


def expert_choice_ffn(h, w_router, w_gate, w_up, w_down):
    bsz, seq, _ = h.shape
    cap = CAPACITY_FACTOR * seq // N_EXPERTS
    aff = jax.nn.softmax((h @ w_router).astype(F32), axis=-1)
    gate, idx = lax.top_k(aff.transpose(0, 2, 1), cap)
    b_idx = jnp.arange(bsz)[:, None, None]
    xg = h[b_idx, idx]
    a = jnp.einsum('becd,edf->becf', xg, w_gate)
    u = jnp.einsum('becd,edf->becf', xg, w_up)
    y = jnp.einsum('becf,efd->becd', jax.nn.silu(a) * u, w_down)
    y = y * gate[..., None].astype(y.dtype)
    return jnp.zeros_like(h).at[b_idx, idx].add(y)


def setup_inputs(seed: int = 0) -> dict:
    key = jax.random.key(seed)
    ks = jax.random.split(key, 32)
    nrm = lambda k, shape, std: jax.random.normal(k, shape, F32) * std
    out_scale = (2 * DEPTH) ** -0.5
    n_idx = jnp.arange(S5_STATE, dtype=F32)
    lam_re = -0.5 + nrm(ks[5], (N_EVEN, 2, S5_GROUPS, S5_STATE), 0.01)
    lam_im = jnp.pi * n_idx + nrm(ks[6], (N_EVEN, 2, S5_GROUPS, S5_STATE), 0.01)
    log_dt = jax.random.uniform(ks[7], (N_EVEN, 2, S5_GROUPS), F32, math.log(DT_MIN), math.log(DT_MAX))
    return {
        'x': nrm(ks[0], (BATCH, SEQ, D_MODEL), 1.0),
        'positions': jnp.broadcast_to(jnp.arange(SEQ, dtype=jnp.int32), (BATCH, SEQ)),
        'norm_mix_g': 1.0 + nrm(ks[1], (DEPTH, D_MODEL), 0.02),
        'norm_ffn_g': 1.0 + nrm(ks[2], (DEPTH, D_MODEL), 0.02),
        'hyb_w_in': nrm(ks[3], (N_EVEN, D_MODEL, MIX_WIDTH), D_MODEL ** -0.5),
        'hyb_w_out': nrm(ks[4], (N_EVEN, MIX_WIDTH, D_MODEL), MIX_WIDTH ** -0.5 * out_scale),
        's5_lam_re': lam_re,
        's5_lam_im': lam_im,
        's5_log_dt': log_dt,
        's5_b_re': nrm(ks[8], (N_EVEN, 2, S5_GROUPS, S5_STATE, S5_GROUP), (2 * S5_GROUP) ** -0.5),
        's5_b_im': nrm(ks[9], (N_EVEN, 2, S5_GROUPS, S5_STATE, S5_GROUP), (2 * S5_GROUP) ** -0.5),
        's5_c_re': nrm(ks[10], (N_EVEN, 2, S5_GROUPS, S5_GROUP, S5_STATE), (2 * S5_STATE) ** -0.5),
        's5_c_im': nrm(ks[11], (N_EVEN, 2, S5_GROUPS, S5_GROUP, S5_STATE), (2 * S5_STATE) ** -0.5),
        's5_d': nrm(ks[12], (N_EVEN, S5_WIDTH), 1.0),
        's5_w_glu': nrm(ks[13], (N_EVEN, S5_WIDTH, S5_WIDTH), S5_WIDTH ** -0.5),
        's5_b_glu': nrm(ks[14], (N_EVEN, S5_WIDTH), 0.01),
        'pool_w': nrm(ks[15], (N_EVEN, len(POOL_WINDOWS), POOL_GROUP, POOL_GROUP), POOL_GROUP ** -0.5),
        'pool_scale': 1.0 + nrm(ks[16], (N_EVEN, POOL_WIDTH), 0.02),
        'attn_w_qkv': nrm(ks[17], (N_ODD, D_MODEL, 2 * QK_WIDTH + ATT_HEADS * ATT_V_DIM), D_MODEL ** -0.5),
        'attn_w_out': nrm(ks[18], (N_ODD, ATT_HEADS * ATT_V_DIM, D_MODEL), (ATT_HEADS * ATT_V_DIM) ** -0.5 * out_scale),
        'attn_q_norm_g': 1.0 + nrm(ks[19], (N_ODD, ATT_HEAD_DIM), 0.02),
        'attn_k_norm_g': 1.0 + nrm(ks[20], (N_ODD, ATT_HEAD_DIM), 0.02),
        'attn_lam_q1': nrm(ks[21], (N_ODD, ATT_HEAD_DIM), 0.1),
        'attn_lam_k1': nrm(ks[22], (N_ODD, ATT_HEAD_DIM), 0.1),
        'attn_lam_q2': nrm(ks[23], (N_ODD, ATT_HEAD_DIM), 0.1),
        'attn_lam_k2': nrm(ks[24], (N_ODD, ATT_HEAD_DIM), 0.1),
        'attn_subln_g': 1.0 + nrm(ks[25], (N_ODD, ATT_V_DIM), 0.02),
        'moe_w_router': nrm(ks[26], (DEPTH, D_MODEL, N_EXPERTS), D_MODEL ** -0.5),
        'moe_w_gate': nrm(ks[27], (DEPTH, N_EXPERTS, D_MODEL, EXPERT_FF), D_MODEL ** -0.5),
        'moe_w_up': nrm(ks[28], (DEPTH, N_EXPERTS, D_MODEL, EXPERT_FF), D_MODEL ** -0.5),
        'moe_w_down': nrm(ks[29], (DEPTH, N_EXPERTS, EXPERT_FF, D_MODEL), EXPERT_FF ** -0.5 * out_scale),
    }


def reference(x, positions, norm_mix_g, norm_ffn_g, hyb_w_in, hyb_w_out, s5_lam_re, s5_lam_im,
              s5_log_dt, s5_b_re, s5_b_im, s5_c_re, s5_c_im, s5_d, s5_w_glu, s5_b_glu, pool_w,
              pool_scale, attn_w_qkv, attn_w_out, attn_q_norm_g, attn_k_norm_g, attn_lam_q1,
              attn_lam_k1, attn_lam_q2, attn_lam_k2, attn_subln_g, moe_w_router, moe_w_gate,
              moe_w_up, moe_w_down):
    cos, sin = rope_tables(positions, ATT_HEAD_DIM)
    h = x
    for layer in range(DEPTH):
        n = rmsnorm(h, norm_mix_g[layer])
        if layer % 2 == 0:
            e = layer // 2
            z = n @ hyb_w_in[e]
            ya = s5_mixer(z[..., :S5_WIDTH], s5_lam_re[e], s5_lam_im[e], s5_log_dt[e],
                          s5_b_re[e], s5_b_im[e], s5_c_re[e], s5_c_im[e], s5_d[e],
                          s5_w_glu[e], s5_b_glu[e])
            yb = pool_mixer(z[..., S5_WIDTH:], pool_w[e], pool_scale[e])
            mix = jnp.concatenate([ya, yb], axis=-1) @ hyb_w_out[e]
        else:
            o = layer // 2
            mix = diff_attention(n, cos, sin, attn_w_qkv[o], attn_w_out[o], attn_q_norm_g[o],
                                 attn_k_norm_g[o], attn_lam_q1[o], attn_lam_k1[o], attn_lam_q2[o],
                                 attn_lam_k2[o], attn_subln_g[o], layer)
        h = h + mix
        h = h + expert_choice_ffn(rmsnorm(h, norm_ffn_g[layer]), moe_w_router[layer],
                                  moe_w_gate[layer], moe_w_up[layer], moe_w_down[layer])
    return h
```

```python
import math
from contextlib import ExitStack
import numpy as np
import concourse.bass as bass
import concourse.mybir as mybir
from concourse.bass_utils import run_bass_kernel_spmd

F32 = mybir.dt.float32
BF16 = mybir.dt.bfloat16
I32 = mybir.dt.int32
U8 = mybir.dt.uint8
AF = mybir.ActivationFunctionType
ALU = mybir.AluOpType
AX = mybir.AxisListType

D = 1024
DC = 8
EPS = 1e-6
TWO_PI = 2.0 * math.pi
CW1 = 6.28125
CW2 = TWO_PI - CW1
MAGIC = 12582912.0


class Dep:
    __slots__ = ("w", "r")

    def __init__(self):
        self.w = None
        self.r = {}


class T:
    def __init__(self, t):
        self.t = t
        self.d = Dep()

    def __getitem__(self, key):
        return self.t[key]


class KB:
    NDMA = 8

    def __init__(self):
        self.nc = bass.Bass("TRN2", target_bir_lowering=False)
        nc = self.nc
        self.eng = {"pe": nc.tensor, "dve": nc.vector, "act": nc.scalar, "pool": nc.gpsimd, "sp": nc.sync}
        self.sems = {}
        self.cnt = {}
        for e in ("pe", "dve", "act", "pool"):
            self.sems[e] = nc.alloc_semaphore("c_" + e)
            self.cnt[e] = 0
        self.dma_pool = {}
        for q in ("sp", "act", "pool"):
            lst = []
            for i in range(self.NDMA):
                nm = "d_%s%d" % (q, i)
                self.sems[nm] = nc.alloc_semaphore(nm)
                self.cnt[nm] = 0
                lst.append(nm)
            self.dma_pool[q] = [lst, 0]
        self.seen = {e: {} for e in self.eng}
        self.ninst = 0

    def _wait(self, e, tok):
        if tok is None:
            return
        s, v = tok
        if e == "pe" and s == "pe":
            return
        if self.seen[e].get(s, 0) >= v:
            return
        self.eng[e].wait_ge(self.sems[s], v)
        self.seen[e][s] = v

    def _pre(self, e, r, w):
        for d in r:
            self._wait(e, d.w)
        for d in w:
            self._wait(e, d.w)
            for s, v in d.r.items():
                self._wait(e, (s, v))

    def _post(self, tok, r, w):
        s, v = tok
        for d in r:
            if d.r.get(s, 0) < v:
                d.r[s] = v
        for d in w:
            d.w = tok
            d.r = {}

    @staticmethod
    def _deps(lst):
        return [x.d if isinstance(x, T) else x for x in lst]

    def op(self, e, fn, r=(), w=()):
        r = self._deps(r)
        w = self._deps(w)
        self._pre(e, r, w)
        inst = fn(self.eng[e])
        self.cnt[e] += 1
        inst.then_inc(self.sems[e], 1)
        self._post((e, self.cnt[e]), r, w)
        self.ninst += 1

    def dma(self, q, fn, r=(), w=()):
        r = self._deps(r)
        w = self._deps(w)
        lst, idx = self.dma_pool[q]
        nm = lst[idx % len(lst)]
        self.dma_pool[q][1] = idx + 1
        if self.cnt[nm]:
            self._wait(q, (nm, self.cnt[nm]))
        self._pre(q, r, w)
        inst = fn(self.eng[q])
        self.cnt[nm] += 16
        inst.then_inc(self.sems[nm], 16)
        self._post((nm, self.cnt[nm]), r, w)
        self.ninst += 1

    def barrier(self, engines=("pe", "dve", "act", "pool", "sp")):
        for e in engines:
            for s, v in self.cnt.items():
                if v:
                    self._wait(e, (s, v))


class Phase:
    def __init__(self, k, name):
        self.k = k
        self.name = name
        self.es = ExitStack()
        self.i = 0

    def __enter__(self):
        self.es.__enter__()
        return self

    def __exit__(self, *a):
        self.k.barrier()
        return self.es.__exit__(*a)

    def sb(self, shape, dt, name=None):
        self.k.ninst_names = getattr(self.k, "ninst_names", 0) + 1
        nm = "%s_%s%d" % (self.name, name or "s", self.k.ninst_names)
        return T(self.es.enter_context(self.k.nc.sbuf_tensor(nm, list(shape), dt)))

    def ps(self, shape, dt, name=None):
        self.k.ninst_names = getattr(self.k, "ninst_names", 0) + 1
        nm = "%s_%s%d" % (self.name, name or "p", self.k.ninst_names)
        return T(self.es.enter_context(self.k.nc.psum_tensor(nm, list(shape), dt)))


class Ring:
    def __init__(self, items):
        self.items = items
        self.i = 0

    def next(self):
        x = self.items[self.i % len(self.items)]
        self.i += 1
        return x


class Cfg:
    def __init__(self, S=16384, DEPTH=4, E=16, FF=2048):
        self.S = S
        self.DEPTH = DEPTH
        self.E = E
        self.FF = FF
        self.CAP = 2 * S // E
        self.NE = (DEPTH + 1) // 2
        self.NO = DEPTH // 2


def mm(k, out, lhsT, rhs, start, stop, r, w):
    k.op("pe", lambda e: e.matmul(out, lhsT, rhs, start=start, stop=stop), r=r, w=w)


def tr(k, out, in_, ident, r, w):
    k.op("pe", lambda e: e.transpose(out, in_, ident), r=r, w=w)


def make_consts(k, P):
    c = {}
    io = P.sb([128, 128], F32, "iota")
    k.op("pool", lambda e: e.iota(io[:], pattern=[[1, 128]], base=0, channel_multiplier=-1,
                                  allow_small_or_imprecise_dtypes=True), w=[io])
    c["idf"] = P.sb([128, 128], F32, "idf")
    k.op("dve", lambda e: e.tensor_scalar(out=c["idf"][:], in0=io[:], scalar1=0.0, scalar2=None,
                                          op0=ALU.is_equal), r=[io], w=[c["idf"]])
    c["idb"] = P.sb([128, 128], BF16, "idb")
    k.op("dve", lambda e: e.tensor_copy(out=c["idb"][:], in_=c["idf"][:]), r=[c["idf"]], w=[c["idb"]])
    c["oneb"] = P.sb([128, 128], BF16, "oneb")
    k.op("dve", lambda e: e.memset(c["oneb"][:], 1.0), w=[c["oneb"]])
    c["onef"] = P.sb([128, 512], F32, "onef")
    k.op("dve", lambda e: e.memset(c["onef"][:], 1.0), w=[c["onef"]])
    return c


def rms_rstd(k, x_ap, xdep, junk, ss, rstd, n, eps=EPS):
    k.op("act", lambda e: e.activation(out=junk[:, 0:n], in_=x_ap, func=AF.Square, accum_out=ss[:, 0:1]),
         r=[xdep], w=[junk, ss])
    k.op("dve", lambda e: e.tensor_scalar(out=ss[:, 0:1], in0=ss[:, 0:1], scalar1=1.0 / n, scalar2=eps,
                                          op0=ALU.mult, op1=ALU.add), r=[ss], w=[ss])
    k.op("act", lambda e: e.activation(out=ss[:, 0:1], in_=ss[:, 0:1], func=AF.Sqrt), r=[ss], w=[ss])
    k.op("dve", lambda e: e.reciprocal(out=rstd[:, 0:1], in_=ss[:, 0:1]), r=[ss], w=[rstd])


def load_w_bf16(k, dst, src_ap, rows_chunks):
    for c in range(rows_chunks):
        k.dma("pool", lambda e: e.dma_start(out=dst[:, c, :], in_=src_ap[c * 128:(c + 1) * 128, :]), w=[dst])


def norm_supertile(k, c, h_ap, t0, nsub, gbc, ring_h, junk, ss, rstd, nb_ring, tp_ring, nT):
    for st in range(nsub):
        ht = ring_h.next()
        r0 = t0 + st * 128
        k.dma("sp", lambda e: e.dma_start(out=ht[:], in_=h_ap[r0:r0 + 128, :]), w=[ht])
        rms_rstd(k, ht[:], ht, junk, ss, rstd, D)
        nb = nb_ring.next()
        k.op("dve", lambda e: e.scalar_tensor_tensor(out=nb[:], in0=ht[:], scalar=rstd[:, 0:1], in1=gbc[:],
                                                     op0=ALU.mult, op1=ALU.mult), r=[ht, rstd, gbc], w=[nb])
        tp = tp_ring.next()
        for dc in range(DC):
            tr(k, tp[:, dc * 128:(dc + 1) * 128], nb[:, dc * 128:(dc + 1) * 128], c["idb"][:], r=[nb, c["idb"]], w=[tp])
        k.op("act", lambda e: e.activation(out=nT[:, :, st * 128:(st + 1) * 128],
                                           in_=tp[:].rearrange("p (c t) -> p c t", c=DC), func=AF.Copy),
             r=[tp], w=[nT])


def bcast_row(k, dst, src_row_ap):
    k.dma("sp", lambda e: e.dma_start(out=dst[:], in_=src_row_ap.to_broadcast([128, src_row_ap.shape[-1]])), w=[dst])


def phase_copy_in(k, cfg, x, out):
    S = cfg.S
    with Phase(k, "cp") as P:
        ring = Ring([P.sb([128, 8192], F32) for _ in range(2)])
        xv = x.rearrange("(p r) d -> p (r d)", p=128)
        ov = out.rearrange("(p r) d -> p (r d)", p=128)
        tot = S // 128 * D
        for o in range(0, tot, 8192):
            n = min(8192, tot - o)
            b = ring.next()
            k.dma("sp", lambda e: e.dma_start(out=b[:, 0:n], in_=xv[:, o:o + n]), w=[b])
            k.dma("sp", lambda e: e.dma_start(out=ov[:, o:o + n], in_=b[:, 0:n]), r=[b])


def phase_even_in(k, cfg, io, layer):
    S = cfg.S
    ev = layer // 2
    h = io["out"]
    with Phase(k, "a1") as P:
        c = make_consts(k, P)
        gbc = P.sb([128, D], F32)
        bcast_row(k, gbc, io["norm_mix_g"][layer:layer + 1, :])
        win = P.sb([128, DC, D], BF16)
        load_w_bf16(k, win, io["hyb_w_in"][ev], DC)
        ring_h = Ring([P.sb([128, D], F32) for _ in range(2)])
        junk = P.sb([128, D], F32)
        ss = P.sb([128, 1], F32)
        rstd = P.sb([128, 1], F32)
        nb_ring = Ring([P.sb([128, D], BF16) for _ in range(2)])
        tp_ring = Ring([P.ps([128, D], BF16) for _ in range(2)])
        TS = min(512, S)
        nT_ring = Ring([P.sb([128, DC, TS], BF16) for _ in range(2)])
        zp_ring = Ring([P.ps([128, TS], F32) for _ in range(3)])
        ub_ring = Ring([P.sb([128, TS], BF16) for _ in range(2)])
        zf_ring = Ring([P.sb([128, TS], F32) for _ in range(2)])
        for t0 in range(0, S, TS):
            nT = nT_ring.next()
            norm_supertile(k, c, h, t0, TS // 128, gbc, ring_h, junk, ss, rstd, nb_ring, tp_ring, nT)
            for cc in range(8):
                zp = zp_ring.next()
                for dc in range(DC):
                    mm(k, zp[:], win[:, dc, cc * 128:(cc + 1) * 128], nT[:, dc, :], dc == 0, dc == DC - 1,
                       r=[win, nT], w=[zp])
                if cc < 4:
                    ub = ub_ring.next()
                    k.op("act", lambda e: e.activation(out=ub[:], in_=zp[:], func=AF.Copy), r=[zp], w=[ub])
                    k.dma("sp", lambda e: e.dma_start(out=io["uT"][cc * 128:(cc + 1) * 128, t0:t0 + TS], in_=ub[:]),
                          r=[ub], w=[io["d_uT"]])
                else:
                    zf = zf_ring.next()
                    k.op("dve", lambda e: e.tensor_copy(out=zf[:], in_=zp[:]), r=[zp], w=[zf])
                    k.dma("sp", lambda e: e.dma_start(out=io["zpT"][(cc - 4) * 128:(cc - 3) * 128, t0:t0 + TS], in_=zf[:]),
                          r=[zf], w=[io["d_zpT"]])


def range_reduce(k, ang, tmp, m, n, shift=0.0):
    if shift != 0.0:
        k.op("dve", lambda e: e.tensor_scalar(out=ang[:, 0:n], in0=ang[:, 0:n], scalar1=shift, scalar2=None, op0=ALU.add),
             r=[ang], w=[ang])
    k.op("dve", lambda e: e.tensor_scalar(out=tmp[:, 0:n], in0=ang[:, 0:n], scalar1=1.0 / TWO_PI, scalar2=MAGIC,
                                          op0=ALU.mult, op1=ALU.add), r=[ang], w=[tmp])
    k.op("dve", lambda e: e.tensor_scalar(out=m[:, 0:n], in0=tmp[:, 0:n], scalar1=MAGIC, scalar2=None, op0=ALU.subtract),
         r=[tmp], w=[m])
    k.op("dve", lambda e: e.scalar_tensor_tensor(out=tmp[:, 0:n], in0=m[:, 0:n], scalar=-CW1, in1=ang[:, 0:n],
                                                 op0=ALU.mult, op1=ALU.add), r=[m, ang], w=[tmp])
    k.op("dve", lambda e: e.scalar_tensor_tensor(out=ang[:, 0:n], in0=m[:, 0:n], scalar=-CW2, in1=tmp[:, 0:n],
                                                 op0=ALU.mult, op1=ALU.add), r=[m, tmp], w=[ang])
    k.op("dve", lambda e: e.tensor_scalar(out=ang[:, 0:n], in0=ang[:, 0:n], scalar1=3.1415925, scalar2=-3.1415925,
                                          op0=ALU.min, op1=ALU.max), r=[ang], w=[ang])


def phase_s5(k, cfg, io, layer):
    S = cfg.S
    ev = layer // 2
    TB = min(512, S)
    NB = S // TB
    with Phase(k, "s5") as P:
        c = make_consts(k, P)
        iot = P.sb([128, TB], F32)
        k.op("pool", lambda e: e.iota(iot[:], pattern=[[1, TB]], base=1, channel_multiplier=0,
                                      allow_small_or_imprecise_dtypes=True), w=[iot])
        cosT = [P.sb([128, TB], F32) for _ in range(4)]
        sinT = [P.sb([128, TB], F32) for _ in range(4)]
        Ar = [P.sb([128, TB], F32) for _ in range(4)]
        Wre = [P.sb([128, 128], BF16) for _ in range(4)]
        Wim = [P.sb([128, 128], BF16) for _ in range(4)]
        Cre = [P.sb([128, 128], F32) for _ in range(4)]
        Cim = [P.sb([128, 128], F32) for _ in range(4)]
        car_re = [P.sb([128, 1], F32) for _ in range(4)]
        car_im = [P.sb([128, 1], F32) for _ in range(4)]
        dsk = P.sb([128, 128], BF16)
        pr = {n: P.sb([128, 1], F32, n) for n in ("lre", "lim", "ldt", "dt", "th", "rho", "r", "are", "aim", "am1",
                                                  "den", "kre", "kim", "t1", "t2", "tm", "mm", "dcol")}
        bre = P.sb([128, 16], F32)
        bim = P.sb([128, 16], F32)
        bbr = P.sb([128, 16], F32)
        bbi = P.sb([128, 16], F32)
        tq = P.sb([128, 16], F32)
        BW = P.sb([128, 128], F32)
        Cn = P.sb([32, 128], F32)
        tmpT = P.sb([128, TB], F32)
        mT = P.sb([128, TB], F32)
        pp = P.ps([128, 128], F32)
        ub_ring = Ring([P.sb([128, TB], BF16) for _ in range(2)])
        bu_re = Ring([P.ps([128, TB], F32) for _ in range(2)])
        bu_im = Ring([P.ps([128, TB], F32) for _ in range(2)])
        yps_ring = Ring([P.ps([128, TB], F32) for _ in range(2)])
        t1 = Ring([P.sb([128, TB], F32) for _ in range(2)])
        t2 = Ring([P.sb([128, TB], F32) for _ in range(2)])
        t3 = Ring([P.sb([128, TB], F32) for _ in range(2)])
        t4 = Ring([P.sb([128, TB], F32) for _ in range(2)])
        xre = Ring([P.sb([128, TB], F32) for _ in range(2)])
        xim = Ring([P.sb([128, TB], F32) for _ in range(2)])
        gre = Ring([P.sb([128, TB], F32) for _ in range(2)])
        gim = Ring([P.sb([128, TB], F32) for _ in range(2)])
        hre = Ring([P.sb([128, TB], F32) for _ in range(3)])
        him = Ring([P.sb([128, TB], F32) for _ in range(3)])
        ysb = Ring([P.sb([128, TB], F32) for _ in range(2)])
        yold = Ring([P.sb([128, TB], F32) for _ in range(2)])

        def col(ap3, dirn, g0):
            return ap3[ev, dirn, g0:g0 + 2, :].rearrange("g (s o) -> (g s) o", o=1)

        def prep_tile(dirn, j, g0, r0):
            k.dma("sp", lambda e: e.dma_start(out=pr["lre"][:], in_=col(io["s5_lam_re"], dirn, g0)), w=[pr["lre"]])
            k.dma("sp", lambda e: e.dma_start(out=pr["lim"][:], in_=col(io["s5_lam_im"], dirn, g0)), w=[pr["lim"]])
            for gl in range(2):
                src = io["s5_log_dt"][ev, dirn:dirn + 1, g0 + gl:g0 + gl + 1]
                k.dma("sp", lambda e: e.dma_start(out=pr["ldt"][gl * 64:(gl + 1) * 64, :], in_=src.to_broadcast([64, 1])),
                      w=[pr["ldt"]])
            k.op("act", lambda e: e.activation(out=pr["dt"][:], in_=pr["ldt"][:], func=AF.Exp), r=[pr["ldt"]], w=[pr["dt"]])
            k.op("dve", lambda e: e.tensor_tensor(out=pr["th"][:], in0=pr["lim"][:], in1=pr["dt"][:], op=ALU.mult),
                 r=[pr["lim"], pr["dt"]], w=[pr["th"]])
            k.op("dve", lambda e: e.tensor_tensor(out=pr["rho"][:], in0=pr["lre"][:], in1=pr["dt"][:], op=ALU.mult),
                 r=[pr["lre"], pr["dt"]], w=[pr["rho"]])
            k.op("act", lambda e: e.activation(out=pr["r"][:], in_=pr["rho"][:], func=AF.Exp), r=[pr["rho"]], w=[pr["r"]])
            k.op("dve", lambda e: e.tensor_scalar(out=sinT[j][:], in0=iot[:], scalar1=pr["th"][:, 0:1], scalar2=None,
                                                  op0=ALU.mult), r=[iot, pr["th"]], w=[sinT[j]])
            k.op("dve", lambda e: e.tensor_copy(out=cosT[j][:], in_=sinT[j][:]), r=[sinT[j]], w=[cosT[j]])
            range_reduce(k, sinT[j], tmpT, mT, TB, 0.0)
            range_reduce(k, cosT[j], tmpT, mT, TB, math.pi / 2)
            k.op("act", lambda e: e.activation(out=sinT[j][:], in_=sinT[j][:], func=AF.Sin), r=[sinT[j]], w=[sinT[j]])
            k.op("act", lambda e: e.activation(out=cosT[j][:], in_=cosT[j][:], func=AF.Sin), r=[cosT[j]], w=[cosT[j]])
            k.op("dve", lambda e: e.tensor_scalar(out=Ar[j][:], in0=c["onef"][:, 0:TB], scalar1=pr["r"][:, 0:1], scalar2=None,
                                                  op0=ALU.mult), r=[c["onef"], pr["r"]], w=[Ar[j]])
            k.op("dve", lambda e: e.tensor_tensor(out=pr["are"][:], in0=pr["r"][:], in1=cosT[j][:, 0:1], op=ALU.mult),
                 r=[pr["r"], cosT[j]], w=[pr["are"]])
            k.op("dve", lambda e: e.tensor_tensor(out=pr["aim"][:], in0=pr["r"][:], in1=sinT[j][:, 0:1], op=ALU.mult),
                 r=[pr["r"], sinT[j]], w=[pr["aim"]])
            k.op("dve", lambda e: e.tensor_scalar(out=pr["am1"][:], in0=pr["are"][:], scalar1=-1.0, scalar2=None, op0=ALU.add),
                 r=[pr["are"]], w=[pr["am1"]])
            k.op("dve", lambda e: e.tensor_tensor(out=pr["t1"][:], in0=pr["lre"][:], in1=pr["lre"][:], op=ALU.mult),
                 r=[pr["lre"]], w=[pr["t1"]])
            k.op("dve", lambda e: e.scalar_tensor_tensor(out=pr["t2"][:], in0=pr["lim"][:], scalar=pr["lim"][:, 0:1],
                                                         in1=pr["t1"][:], op0=ALU.mult, op1=ALU.add),
                 r=[pr["lim"], pr["t1"]], w=[pr["t2"]])
            k.op("dve", lambda e: e.reciprocal(out=pr["den"][:], in_=pr["t2"][:]), r=[pr["t2"]], w=[pr["den"]])
            k.op("dve", lambda e: e.tensor_tensor(out=pr["t1"][:], in0=pr["am1"][:], in1=pr["lre"][:], op=ALU.mult),
                 r=[pr["am1"], pr["lre"]], w=[pr["t1"]])
            k.op("dve", lambda e: e.scalar_tensor_tensor(out=pr["t2"][:], in0=pr["aim"][:], scalar=pr["lim"][:, 0:1],
                                                         in1=pr["t1"][:], op0=ALU.mult, op1=ALU.add),
                 r=[pr["aim"], pr["lim"], pr["t1"]], w=[pr["t2"]])
            k.op("dve", lambda e: e.tensor_tensor(out=pr["kre"][:], in0=pr["t2"][:], in1=pr["den"][:], op=ALU.mult),
                 r=[pr["t2"], pr["den"]], w=[pr["kre"]])
            k.op("dve", lambda e: e.tensor_tensor(out=pr["t1"][:], in0=pr["am1"][:], in1=pr["lim"][:], op=ALU.mult),
                 r=[pr["am1"], pr["lim"]], w=[pr["t1"]])
            k.op("dve", lambda e: e.scalar_tensor_tensor(out=pr["t2"][:], in0=pr["aim"][:], scalar=pr["lre"][:, 0:1],
                                                         in1=pr["t1"][:], op0=ALU.mult, op1=ALU.subtract),
                 r=[pr["aim"], pr["lre"], pr["t1"]], w=[pr["t2"]])
            k.op("dve", lambda e: e.tensor_tensor(out=pr["kim"][:], in0=pr["t2"][:], in1=pr["den"][:], op=ALU.mult),
                 r=[pr["t2"], pr["den"]], w=[pr["kim"]])
            k.dma("sp", lambda e: e.dma_start(out=bre[:], in_=io["s5_b_re"][ev, dirn, g0:g0 + 2].rearrange("g s c -> (g s) c")),
                  w=[bre])
            k.dma("sp", lambda e: e.dma_start(out=bim[:], in_=io["s5_b_im"][ev, dirn, g0:g0 + 2].rearrange("g s c -> (g s) c")),
                  w=[bim])
            k.op("dve", lambda e: e.tensor_scalar(out=tq[:], in0=bim[:], scalar1=pr["kim"][:, 0:1], scalar2=None, op0=ALU.mult),
                 r=[bim, pr["kim"]], w=[tq])
            k.op("dve", lambda e: e.scalar_tensor_tensor(out=bbr[:], in0=bre[:], scalar=pr["kre"][:, 0:1], in1=tq[:],
                                                         op0=ALU.mult, op1=ALU.subtract), r=[bre, pr["kre"], tq], w=[bbr])
            k.op("dve", lambda e: e.tensor_scalar(out=tq[:], in0=bre[:], scalar1=pr["kim"][:, 0:1], scalar2=None, op0=ALU.mult),
                 r=[bre, pr["kim"]], w=[tq])
            k.op("dve", lambda e: e.scalar_tensor_tensor(out=bbi[:], in0=bim[:], scalar=pr["kre"][:, 0:1], in1=tq[:],
                                                         op0=ALU.mult, op1=ALU.add), r=[bim, pr["kre"], tq], w=[bbi])
            for src, dst in ((bbr, Wre[j]), (bbi, Wim[j])):
                k.op("dve", lambda e: e.memset(BW[:], 0.0), w=[BW])
                for gl in range(2):
                    k.op("dve", lambda e: e.tensor_copy(out=BW[gl * 64:(gl + 1) * 64, r0 + gl * 16:r0 + gl * 16 + 16],
                                                        in_=src[gl * 64:(gl + 1) * 64, :]), r=[src], w=[BW])
                tr(k, pp[:], BW[:], c["idf"][:], r=[BW, c["idf"]], w=[pp])
                k.op("dve", lambda e: e.tensor_copy(out=dst[:], in_=pp[:]), r=[pp], w=[dst])
            for src_name, dst, sgn in (("s5_c_re", Cre[j], 1.0), ("s5_c_im", Cim[j], -1.0)):
                k.op("dve", lambda e: e.memset(Cn[:], 0.0), w=[Cn])
                for gl in range(2):
                    k.dma("sp", lambda e: e.dma_start(out=Cn[gl * 16:(gl + 1) * 16, gl * 64:(gl + 1) * 64],
                                                      in_=io[src_name][ev, dirn, g0 + gl]), w=[Cn])
                tr(k, pp[:, 0:32], Cn[:], c["idf"][0:32, 0:32], r=[Cn, c["idf"]], w=[pp])
                k.op("dve", lambda e: e.memset(dst[:], 0.0), w=[dst])
                k.op("dve", lambda e: e.tensor_scalar(out=dst[:, r0:r0 + 32], in0=pp[:, 0:32], scalar1=sgn, scalar2=None,
                                                      op0=ALU.mult), r=[pp], w=[dst])
            k.op("dve", lambda e: e.memset(car_re[j][:], 0.0), w=[car_re[j]])
            k.op("dve", lambda e: e.memset(car_im[j][:], 0.0), w=[car_im[j]])

        for dirn in range(2):
            rev = dirn == 1

            def V(ap):
                return ap[:, ::-1] if rev else ap
            for cc in range(4):
                for j in range(4):
                    prep_tile(dirn, j, cc * 8 + 2 * j, 32 * j)
                if not rev:
                    k.dma("sp", lambda e: e.dma_start(out=pr["dcol"][:],
                                                      in_=io["s5_d"][ev:ev + 1, cc * 128:(cc + 1) * 128].rearrange("o c -> c o")),
                          w=[pr["dcol"]])
                    k.op("dve", lambda e: e.tensor_scalar(out=dsk[:], in0=c["idf"][:], scalar1=pr["dcol"][:, 0:1], scalar2=None,
                                                          op0=ALU.mult), r=[c["idf"], pr["dcol"]], w=[dsk])
                blks = list(range(NB))
                if rev:
                    blks = blks[::-1]
                for b in blks:
                    ub = ub_ring.next()
                    k.dma("sp", lambda e: e.dma_start(out=ub[:], in_=io["uT"][cc * 128:(cc + 1) * 128, b * TB:(b + 1) * TB]),
                          r=[io["d_uT"]], w=[ub])
                    yps = yps_ring.next()
                    hs = []
                    for j in range(4):
                        pre, pim = bu_re.next(), bu_im.next()
                        mm(k, pre[:], Wre[j][:], ub[:], True, True, r=[Wre[j], ub], w=[pre])
                        mm(k, pim[:], Wim[j][:], ub[:], True, True, r=[Wim[j], ub], w=[pim])
                        a1, a2, a3, a4 = t1.next(), t2.next(), t3.next(), t4.next()
                        cs, sn = V(cosT[j][:]), V(sinT[j][:])
                        k.op("dve", lambda e: e.tensor_tensor(out=a1[:], in0=pre[:], in1=cs, op=ALU.mult), r=[pre, cosT[j]], w=[a1])
                        k.op("dve", lambda e: e.tensor_tensor(out=a2[:], in0=pim[:], in1=sn, op=ALU.mult), r=[pim, sinT[j]], w=[a2])
                        k.op("dve", lambda e: e.tensor_tensor(out=a3[:], in0=pim[:], in1=cs, op=ALU.mult), r=[pim, cosT[j]], w=[a3])
                        k.op("dve", lambda e: e.tensor_tensor(out=a4[:], in0=pre[:], in1=sn, op=ALU.mult), r=[pre, sinT[j]], w=[a4])
                        xr, xi = xre.next(), xim.next()
                        k.op("pool", lambda e: e.tensor_tensor(out=xr[:], in0=a1[:], in1=a2[:], op=ALU.add), r=[a1, a2], w=[xr])
                        k.op("pool", lambda e: e.tensor_tensor(out=xi[:], in0=a3[:], in1=a4[:], op=ALU.subtract), r=[a3, a4], w=[xi])
                        gr, gi = gre.next(), gim.next()
                        k.op("dve", lambda e: e.tensor_tensor_scan(out=V(gr[:]), data0=Ar[j][:], data1=V(xr[:]),
                                                                   initial=car_re[j][:, 0:1], op0=ALU.mult, op1=ALU.add),
                             r=[Ar[j], xr, car_re[j]], w=[gr])
                        k.op("dve", lambda e: e.tensor_tensor_scan(out=V(gi[:]), data0=Ar[j][:], data1=V(xi[:]),
                                                                   initial=car_im[j][:, 0:1], op0=ALU.mult, op1=ALU.add),
                             r=[Ar[j], xi, car_im[j]], w=[gi])
                        b1, b2, b3, b4 = t1.next(), t2.next(), t3.next(), t4.next()
                        k.op("pool", lambda e: e.tensor_tensor(out=b1[:], in0=gr[:], in1=cs, op=ALU.mult), r=[gr, cosT[j]], w=[b1])
                        k.op("pool", lambda e: e.tensor_tensor(out=b2[:], in0=gi[:], in1=sn, op=ALU.mult), r=[gi, sinT[j]], w=[b2])
                        k.op("pool", lambda e: e.tensor_tensor(out=b3[:], in0=gr[:], in1=sn, op=ALU.mult), r=[gr, sinT[j]], w=[b3])
                        k.op("pool", lambda e: e.tensor_tensor(out=b4[:], in0=gi[:], in1=cs, op=ALU.mult), r=[gi, cosT[j]], w=[b4])
                        hr, hi = hre.next(), him.next()
                        k.op("dve", lambda e: e.tensor_tensor(out=hr[:], in0=b1[:], in1=b2[:], op=ALU.subtract), r=[b1, b2], w=[hr])
                        k.op("dve", lambda e: e.tensor_tensor(out=hi[:], in0=b3[:], in1=b4[:], op=ALU.add), r=[b3, b4], w=[hi])
                        last = 0 if rev else TB - 1
                        k.op("dve", lambda e: e.tensor_copy(out=car_re[j][:], in_=hr[:, last:last + 1]), r=[hr], w=[car_re[j]])
                        k.op("dve", lambda e: e.tensor_copy(out=car_im[j][:], in_=hi[:, last:last + 1]), r=[hi], w=[car_im[j]])
                        hs.append((hr, hi))
                        if len(hs) == 2 or j == 3:
                            pass
                        mm(k, yps[:], Cre[j][:], hr[:], j == 0, False, r=[Cre[j], hr], w=[yps])
                        mm(k, yps[:], Cim[j][:], hi[:], False, (j == 3) and rev, r=[Cim[j], hi], w=[yps])
                    ys = ysb.next()
                    if not rev:
                        mm(k, yps[:], dsk[:], ub[:], False, True, r=[dsk, ub], w=[yps])
                        k.op("act", lambda e: e.activation(out=ys[:], in_=yps[:], func=AF.Copy), r=[yps], w=[ys])
                    else:
                        yo = yold.next()
                        k.dma("sp", lambda e: e.dma_start(out=yo[:], in_=io["yT"][cc * 128:(cc + 1) * 128, b * TB:(b + 1) * TB]),
                              r=[io["d_yT"]], w=[yo])
                        k.op("dve", lambda e: e.tensor_tensor(out=ys[:], in0=yps[:], in1=yo[:], op=ALU.add), r=[yps, yo], w=[ys])
                    k.dma("sp", lambda e: e.dma_start(out=io["yT"][cc * 128:(cc + 1) * 128, b * TB:(b + 1) * TB], in_=ys[:]),
                          r=[ys], w=[io["d_yT"]])


def phase_even_out(k, cfg, io, layer):
    S = cfg.S
    ev = layer // 2
    h = io["out"]
    TS = min(512, S)
    HALO = 8
    with Phase(k, "a3") as P:
        c = make_consts(k, P)
        wglu = P.sb([128, 4, 512], BF16)
        load_w_bf16(k, wglu, io["s5_w_glu"][ev], 4)
        wout = P.sb([128, 8, D], BF16)
        load_w_bf16(k, wout, io["hyb_w_out"][ev], 8)
        pw = P.sb([128, 4, 128], BF16)
        for gi in range(4):
            k.dma("pool", lambda e: e.dma_start(out=pw[:, gi, :], in_=io["pool_w"][ev, gi]), w=[pw])
        bglu = P.sb([128, 4], F32)
        k.dma("sp", lambda e: e.dma_start(out=bglu[:], in_=io["s5_b_glu"][ev:ev + 1, :].rearrange("o (c p) -> p (o c)", p=128)),
              w=[bglu])
        psc = P.sb([128, 4], F32)
        k.dma("sp", lambda e: e.dma_start(out=psc[:], in_=io["pool_scale"][ev:ev + 1, :].rearrange("o (c p) -> p (o c)", p=128)),
              w=[psc])
        iot = P.sb([128, TS], F32)
        k.op("pool", lambda e: e.iota(iot[:], pattern=[[1, TS]], base=0, channel_multiplier=0,
                                      allow_small_or_imprecise_dtypes=True), w=[iot])
        yt_ring = Ring([P.sb([128, TS], F32) for _ in range(2)])
        yg = P.sb([128, 4, TS], F32)
        ygb = P.sb([128, 4, TS], BF16)
        catT = P.sb([128, 8, TS], BF16)
        glp = Ring([P.ps([128, TS], F32) for _ in range(2)])
        sg = Ring([P.sb([128, TS], F32) for _ in range(2)])
        W = TS + 2 * HALO
        zv = Ring([P.sb([128, W], F32) for _ in range(2)])
        sA = P.sb([128, W], F32)
        sB = P.sb([128, W], F32)
        hi_t = P.sb([128, TS], F32)
        lo_t = P.sb([128, TS], F32)
        pT = Ring([P.sb([128, TS], BF16) for _ in range(2)])
        mxp = Ring([P.ps([128, 512], F32) for _ in range(2)])
        ht_ring = Ring([P.sb([128, D], F32) for _ in range(2)])
        for t0 in range(0, S, TS):
            for cc in range(4):
                yt = yt_ring.next()
                k.dma("sp", lambda e: e.dma_start(out=yt[:], in_=io["yT"][cc * 128:(cc + 1) * 128, t0:t0 + TS]),
                      r=[io["d_yT"]], w=[yt])
                k.op("act", lambda e: e.activation(out=yg[:, cc, :], in_=yt[:], func=AF.Gelu), r=[yt], w=[yg])
            k.op("dve", lambda e: e.tensor_copy(out=ygb[:], in_=yg[:]), r=[yg], w=[ygb])
            for c2 in range(4):
                gp = glp.next()
                for cc in range(4):
                    mm(k, gp[:], wglu[:, cc, c2 * 128:(c2 + 1) * 128], ygb[:, cc, :], cc == 0, cc == 3, r=[wglu, ygb], w=[gp])
                s = sg.next()
                k.op("act", lambda e: e.activation(out=s[:], in_=gp[:], func=AF.Sigmoid, bias=bglu[:, c2:c2 + 1]),
                     r=[gp, bglu], w=[s])
                k.op("dve", lambda e: e.tensor_tensor(out=catT[:, c2, :], in0=yg[:, c2, :], in1=s[:], op=ALU.mult),
                     r=[yg, s], w=[catT])
            for gi, win in enumerate((2, 4, 8, 16)):
                z = zv.next()
                lo = max(t0 - HALO, 0)
                hi = min(t0 + TS + HALO, S)
                k.op("pool", lambda e: e.memset(z[:], 0.0), w=[z])
                k.dma("sp", lambda e: e.dma_start(out=z[:, lo - (t0 - HALO):hi - (t0 - HALO)],
                                                  in_=io["zpT"][gi * 128:(gi + 1) * 128, lo:hi]), r=[io["d_zpT"]], w=[z])
                k.op("dve", lambda e: e.memset(sA[:], 0.0), w=[sA])
                k.op("dve", lambda e: e.tensor_tensor(out=sA[:, 1:W], in0=z[:, 0:W - 1], in1=z[:, 1:W], op=ALU.add), r=[z], w=[sA])
                cur, oth = sA, sB
                half = 1
                while 2 * half < win:
                    sh = half
                    k.op("dve", lambda e: e.memset(oth[:], 0.0), w=[oth])
                    k.op("dve", lambda e: e.tensor_tensor(out=oth[:, sh:W - sh], in0=cur[:, 0:W - 2 * sh], in1=cur[:, 2 * sh:W],
                                                          op=ALU.add), r=[cur], w=[oth])
                    cur, oth = oth, cur
                    half *= 2
                k.op("dve", lambda e: e.tensor_scalar(out=hi_t[:], in0=iot[:], scalar1=float(t0 + win // 2), scalar2=float(S),
                                                      op0=ALU.add, op1=ALU.min), r=[iot], w=[hi_t])
                k.op("dve", lambda e: e.tensor_scalar(out=lo_t[:], in0=iot[:], scalar1=float(t0 - win // 2), scalar2=0.0,
                                                      op0=ALU.add, op1=ALU.max), r=[iot], w=[lo_t])
                k.op("dve", lambda e: e.tensor_tensor(out=hi_t[:], in0=hi_t[:], in1=lo_t[:], op=ALU.subtract),
                     r=[hi_t, lo_t], w=[hi_t])
                k.op("dve", lambda e: e.reciprocal(out=hi_t[:], in_=hi_t[:]), r=[hi_t], w=[hi_t])
                k.op("dve", lambda e: e.tensor_tensor(out=lo_t[:], in0=cur[:, HALO:HALO + TS], in1=hi_t[:], op=ALU.mult),
                     r=[cur, hi_t], w=[lo_t])
                p_ = pT.next()
                k.op("dve", lambda e: e.tensor_tensor(out=p_[:], in0=lo_t[:], in1=z[:, HALO:HALO + TS], op=ALU.subtract),
                     r=[lo_t, z], w=[p_])
                gp = glp.next()
                mm(k, gp[:], pw[:, gi, :], p_[:], True, True, r=[pw, p_], w=[gp])
                k.op("act", lambda e: e.activation(out=catT[:, 4 + gi, :], in_=gp[:], func=AF.Copy, scale=psc[:, gi:gi + 1]),
                     r=[gp, psc], w=[catT])
            for st in range(TS // 128):
                ht = ht_ring.next()
                r0 = t0 + st * 128
                k.dma("sp", lambda e: e.dma_start(out=ht[:], in_=h[r0:r0 + 128, :]), r=[io["d_h"]], w=[ht])
                for hf in range(2):
                    mp = mxp.next()
                    for c8 in range(8):
                        mm(k, mp[:], catT[:, c8, st * 128:(st + 1) * 128], wout[:, c8, hf * 512:(hf + 1) * 512],
                           c8 == 0, c8 == 7, r=[catT, wout], w=[mp])
                    k.op("dve", lambda e: e.tensor_tensor(out=ht[:, hf * 512:(hf + 1) * 512], in0=mp[:],
                                                          in1=ht[:, hf * 512:(hf + 1) * 512], op=ALU.add), r=[mp, ht], w=[ht])
                k.dma("sp", lambda e: e.dma_start(out=h[r0:r0 + 128, :], in_=ht[:]), r=[ht], w=[io["d_h"]])


def phase_moe(k, cfg, io, layer):
    S, E, FF, CAP = cfg.S, cfg.E, cfg.FF, cfg.CAP
    h = io["out"]
    FC = FF // 128
    NJT = CAP // 128
    with Phase(k, "m1") as P:
        c = make_consts(k, P)
        gbc = P.sb([128, D], F32)
        bcast_row(k, gbc, io["norm_ffn_g"][layer:layer + 1, :])
        wr = P.sb([128, DC, E], F32)
        k.dma("sp", lambda e: e.dma_start(out=wr[:], in_=io["moe_w_router"][layer].rearrange("(c p) e -> p c e", p=128)), w=[wr])
        ring_h = Ring([P.sb([128, D], F32) for _ in range(2)])
        junk = P.sb([128, D], F32)
        ss = P.sb([128, 1], F32)
        rstd = P.sb([128, 1], F32)
        n32 = Ring([P.sb([128, D], F32) for _ in range(2)])
        nbf = Ring([P.sb([128, D], BF16) for _ in range(2)])
        tp32 = Ring([P.ps([128, D], F32) for _ in range(2)])
        nT32 = Ring([P.sb([128, D], F32) for _ in range(2)])
        lgp = Ring([P.ps([128, E], F32) for _ in range(2)])
        mx = P.sb([128, 1], F32)
        se = P.sb([128, 1], F32)
        ex = P.sb([128, E], F32)
        aff = Ring([P.sb([128, E], F32) for _ in range(2)])
        atp = Ring([P.ps([E, 128], F32) for _ in range(2)])
        affT = P.sb([E, S], F32)
        for t0 in range(0, S, 128):
            ht = ring_h.next()
            k.dma("sp", lambda e: e.dma_start(out=ht[:], in_=h[t0:t0 + 128, :]), r=[io["d_h"]], w=[ht])
            rms_rstd(k, ht[:], ht, junk, ss, rstd, D)
            n = n32.next()
            k.op("dve", lambda e: e.scalar_tensor_tensor(out=n[:], in0=ht[:], scalar=rstd[:, 0:1], in1=gbc[:],
                                                         op0=ALU.mult, op1=ALU.mult), r=[ht, rstd, gbc], w=[n])
            nb = nbf.next()
            k.op("act", lambda e: e.activation(out=nb[:], in_=n[:], func=AF.Copy), r=[n], w=[nb])
            k.dma("sp", lambda e: e.dma_start(out=io["hn"][t0:t0 + 128, :], in_=nb[:]), r=[nb], w=[io["d_hn"]])
            tp = tp32.next()
            for dc in range(DC):
                tr(k, tp[:, dc * 128:(dc + 1) * 128], n[:, dc * 128:(dc + 1) * 128], c["idf"][:], r=[n, c["idf"]], w=[tp])
            nt = nT32.next()
            k.op("dve", lambda e: e.tensor_copy(out=nt[:], in_=tp[:]), r=[tp], w=[nt])
            lg = lgp.next()
            for dc in range(DC):
                mm(k, lg[:], nt[:, dc * 128:(dc + 1) * 128], wr[:, dc, :], dc == 0, dc == DC - 1, r=[nt, wr], w=[lg])
            k.op("dve", lambda e: e.tensor_reduce(out=mx[:], in_=lg[:], op=ALU.max, axis=AX.X), r=[lg], w=[mx])
            k.op("dve", lambda e: e.tensor_scalar(out=mx[:], in0=mx[:], scalar1=-1.0, scalar2=None, op0=ALU.mult), r=[mx], w=[mx])
            k.op("act", lambda e: e.activation(out=ex[:], in_=lg[:], func=AF.Exp, bias=mx[:, 0:1], accum_out=se[:, 0:1]),
                 r=[lg, mx], w=[ex, se])
            k.op("dve", lambda e: e.reciprocal(out=se[:], in_=se[:]), r=[se], w=[se])
            a = aff.next()
            k.op("dve", lambda e: e.tensor_scalar(out=a[:], in0=ex[:], scalar1=se[:, 0:1], scalar2=None, op0=ALU.mult),
                 r=[ex, se], w=[a])
            k.dma("sp", lambda e: e.dma_start(out=io["affd"][t0:t0 + 128, :], in_=a[:]), r=[a], w=[io["d_affd"]])
            ap_ = atp.next()
            tr(k, ap_[:], a[:], c["idf"][:], r=[a, c["idf"]], w=[ap_])
            k.op("dve", lambda e: e.tensor_copy(out=affT[:, t0:t0 + 128], in_=ap_[:]), r=[ap_], w=[affT])
        k.dma("sp", lambda e: e.dma_start(out=io["csd"][:, :], in_=affT[:]), r=[affT], w=[io["d_csd"]])

    with Phase(k, "m2") as P:
        affT = P.sb([E, S], F32)
        k.dma("sp", lambda e: e.dma_start(out=affT[:], in_=io["csd"][:, :]), r=[io["d_csd"]], w=[affT])
        msk = P.sb([E, S], BF16)
        lo = P.sb([E, 1], F32)
        hi = P.sb([E, 1], F32)
        mid = P.sb([E, 1], F32)
        cnt = P.sb([E, 1], F32)
        sel = P.sb([E, 1], U8)
        nsel = P.sb([E, 1], U8)
        k.op("dve", lambda e: e.memset(lo[:], 0.0), w=[lo])
        k.op("dve", lambda e: e.memset(hi[:], 1.0), w=[hi])
        for it in range(40):
            k.op("dve", lambda e: e.tensor_tensor(out=mid[:], in0=lo[:], in1=hi[:], op=ALU.add), r=[lo, hi], w=[mid])
            k.op("dve", lambda e: e.tensor_scalar(out=mid[:], in0=mid[:], scalar1=0.5, scalar2=None, op0=ALU.mult), r=[mid], w=[mid])
            k.op("dve", lambda e: e.tensor_scalar(out=msk[:], in0=affT[:], scalar1=mid[:, 0:1], scalar2=0.0, op0=ALU.is_gt,
                                                  op1=ALU.add, accum_out=cnt[:, 0:1]), r=[affT, mid], w=[msk, cnt])
            k.op("dve", lambda e: e.tensor_scalar(out=sel[:], in0=cnt[:], scalar1=float(CAP), scalar2=None, op0=ALU.is_ge),
                 r=[cnt], w=[sel])
            k.op("dve", lambda e: e.tensor_scalar(out=nsel[:], in0=cnt[:], scalar1=float(CAP), scalar2=None, op0=ALU.is_lt),
                 r=[cnt], w=[nsel])
            k.op("dve", lambda e: e.copy_predicated(out=lo[:], mask=sel[:], data=mid[:]), r=[sel, mid], w=[lo])
            k.op("dve", lambda e: e.copy_predicated(out=hi[:], mask=nsel[:], data=mid[:]), r=[nsel, mid], w=[hi])
        k.op("dve", lambda e: e.tensor_scalar(out=msk[:], in0=affT[:], scalar1=lo[:, 0:1], scalar2=None, op0=ALU.is_gt),
             r=[affT, lo], w=[msk])
        k.op("dve", lambda e: e.tensor_tensor_scan(out=affT[:], data0=msk[:], data1=msk[:], initial=0.0,
                                                   op0=ALU.add, op1=ALU.max), r=[msk], w=[affT])
        k.dma("sp", lambda e: e.dma_start(out=io["csd"][:, :], in_=affT[:]), r=[affT], w=[io["d_csd"]])
    with Phase(k, "m3") as P:
        HS = min(S, 8192)
        cb = Ring([P.sb([128, HS], F32) for _ in range(2)])
        junk = P.sb([128, HS], U8)
        jall = P.sb([128, NJT], F32)
        k.op("pool", lambda e: e.iota(jall[:], pattern=[[128, NJT]], base=0, channel_multiplier=1,
                                      allow_small_or_imprecise_dtypes=True), w=[jall])
        part = P.sb([128, 2], F32)
        idxf = P.sb([128, 1], F32)
        idx_i = P.sb([128, E * NJT], I32)
        for ex_ in range(E):
            halves = []
            for hs in range(0, S, HS):
                b = cb.next()
                k.dma("sp", lambda e: e.dma_start(out=b[:], in_=io["csd"][ex_:ex_ + 1, hs:hs + HS].to_broadcast([128, HS])),
                      r=[io["d_csd"]], w=[b])
                halves.append(b)
            for jt in range(NJT):
                for hi_, b in enumerate(halves):
                    k.op("dve", lambda e: e.tensor_scalar(out=junk[:], in0=b[:], scalar1=jall[:, jt:jt + 1], scalar2=0.0,
                                                          op0=ALU.is_le, op1=ALU.add, accum_out=part[:, hi_:hi_ + 1]),
                         r=[b, jall], w=[junk, part])
                col = ex_ * NJT + jt
                if len(halves) == 2:
                    k.op("dve", lambda e: e.tensor_tensor(out=idxf[:], in0=part[:, 0:1], in1=part[:, 1:2], op=ALU.add),
                         r=[part], w=[idxf])
                    src = idxf
                else:
                    src = part
                k.op("dve", lambda e: e.tensor_scalar(out=idxf[:], in0=src[:, 0:1], scalar1=float(S - 1), scalar2=None, op0=ALU.min),
                     r=[src], w=[idxf])
                k.op("dve", lambda e: e.tensor_copy(out=idx_i[:, col:col + 1], in_=idxf[:]), r=[idxf], w=[idx_i])
        k.dma("sp", lambda e: e.dma_start(out=io["idxd"][:, :], in_=idx_i[:]), r=[idx_i], w=[io["d_idxd"]])

    with Phase(k, "m4") as P:
        c = make_consts(k, P)
        idx_i = P.sb([128, E * NJT], I32)
        k.dma("sp", lambda e: e.dma_start(out=idx_i[:], in_=io["idxd"][:, :]), r=[io["d_idxd"]], w=[idx_i])
        wg = P.sb([128, DC, FF], BF16)
        wu = P.sb([128, DC, FF], BF16)
        wd = P.sb([128, FC, D], BF16)
        GS = min(4, NJT)
        NG = NJT // GS
        xg = Ring([P.sb([128, D], BF16) for _ in range(3)])
        ar = Ring([P.sb([128, E], F32) for _ in range(2 * GS)])
        tpx = Ring([P.ps([128, D], BF16) for _ in range(2)])
        xgT = P.sb([128, DC, GS * 128], BF16)
        actT = P.sb([128, FC, GS * 128], BF16)
        pa = Ring([P.ps([128, GS * 128], F32) for _ in range(2)])
        pu = Ring([P.ps([128, GS * 128], F32) for _ in range(2)])
        sa = Ring([P.sb([128, GS * 128], F32) for _ in range(2)])
        py = Ring([P.ps([128, 512], F32) for _ in range(2)])
        ys = Ring([P.sb([128, D], F32) for _ in range(2)])
        for ex_ in range(E):
            load_w_bf16(k, wg, io["moe_w_gate"][layer, ex_], DC)
            load_w_bf16(k, wu, io["moe_w_up"][layer, ex_], DC)
            load_w_bf16(k, wd, io["moe_w_down"][layer, ex_], FC)
            for g in range(NG):
                gates = []
                for st in range(GS):
                    col = ex_ * NJT + g * GS + st
                    x_ = xg.next()
                    k.dma("pool", lambda e: e.indirect_dma_start(
                        out=x_[:], out_offset=None, in_=io["hn"],
                        in_offset=bass.IndirectOffsetOnAxis(ap=idx_i[:, col:col + 1], axis=0)),
                        r=[idx_i, io["d_hn"]], w=[x_])
                    a_ = ar.next()
                    k.dma("pool", lambda e: e.indirect_dma_start(
                        out=a_[:], out_offset=None, in_=io["affd"],
                        in_offset=bass.IndirectOffsetOnAxis(ap=idx_i[:, col:col + 1], axis=0)),
                        r=[idx_i, io["d_affd"]], w=[a_])
                    gates.append(a_)
                    tp = tpx.next()
                    for dc in range(DC):
                        tr(k, tp[:, dc * 128:(dc + 1) * 128], x_[:, dc * 128:(dc + 1) * 128], c["idb"][:], r=[x_, c["idb"]], w=[tp])
                    k.op("act", lambda e: e.activation(out=xgT[:, :, st * 128:(st + 1) * 128],
                                                       in_=tp[:].rearrange("p (c t) -> p c t", c=DC), func=AF.Copy),
                         r=[tp], w=[xgT])
                for fc in range(FC):
                    a_p, u_p = pa.next(), pu.next()
                    for dc in range(DC):
                        mm(k, a_p[:], wg[:, dc, fc * 128:(fc + 1) * 128], xgT[:, dc, :], dc == 0, dc == DC - 1, r=[wg, xgT], w=[a_p])
                    for dc in range(DC):
                        mm(k, u_p[:], wu[:, dc, fc * 128:(fc + 1) * 128], xgT[:, dc, :], dc == 0, dc == DC - 1, r=[wu, xgT], w=[u_p])
                    s_ = sa.next()
                    k.op("act", lambda e: e.activation(out=s_[:], in_=a_p[:], func=AF.Silu), r=[a_p], w=[s_])
                    k.op("dve", lambda e: e.tensor_tensor(out=actT[:, fc, :], in0=u_p[:], in1=s_[:], op=ALU.mult),
                         r=[u_p, s_], w=[actT])
                for st in range(GS):
                    col = ex_ * NJT + g * GS + st
                    y_ = ys.next()
                    for hf in range(2):
                        yp = py.next()
                        for fc in range(FC):
                            mm(k, yp[:], actT[:, fc, st * 128:(st + 1) * 128], wd[:, fc, hf * 512:(hf + 1) * 512],
                               fc == 0, fc == FC - 1, r=[actT, wd], w=[yp])
                        k.op("dve", lambda e: e.tensor_scalar(out=y_[:, hf * 512:(hf + 1) * 512], in0=yp[:],
                                                              scalar1=gates[st][:, ex_:ex_ + 1], scalar2=None, op0=ALU.mult),
                             r=[yp, gates[st]], w=[y_])
                    k.dma("pool", lambda e: e.indirect_dma_start(
                        out=h, out_offset=bass.IndirectOffsetOnAxis(ap=idx_i[:, col:col + 1], axis=0),
                        in_=y_[:], in_offset=None, compute_op=ALU.add), r=[y_, idx_i], w=[io["d_h"]])


def phase_attn(k, cfg, io, layer):
    S = cfg.S
    od = layer // 2
    h = io["out"]
    TS = min(512, S)
    NST = TS // 128
    lam_init = 0.8 - 0.6 * math.exp(-0.3 * layer)
    with Phase(k, "b1") as P:
        c = make_consts(k, P)
        gbc = P.sb([128, D], F32)
        bcast_row(k, gbc, io["norm_mix_g"][layer:layer + 1, :])
        wqkv = P.sb([128, DC, 3 * D], BF16)
        load_w_bf16(k, wqkv, io["attn_w_qkv"][od], DC)
        gq = P.sb([128, 64], F32)
        gk = P.sb([128, 64], F32)
        bcast_row(k, gq, io["attn_q_norm_g"][od:od + 1, :])
        bcast_row(k, gk, io["attn_k_norm_g"][od:od + 1, :])
        k.op("dve", lambda e: e.tensor_scalar(out=gq[:], in0=gq[:], scalar1=0.125, scalar2=None, op0=ALU.mult), r=[gq], w=[gq])
        inv = P.sb([128, 32], F32)
        k.op("pool", lambda e: e.iota(inv[:], pattern=[[1, 32]], base=0, channel_multiplier=0,
                                      allow_small_or_imprecise_dtypes=True), w=[inv])
        k.op("act", lambda e: e.activation(out=inv[:], in_=inv[:], func=AF.Exp, scale=-math.log(10000.0) / 32.0), r=[inv], w=[inv])
        ring_h = Ring([P.sb([128, D], F32) for _ in range(2)])
        junk = P.sb([128, D], F32)
        ss = P.sb([128, 1], F32)
        rstd = P.sb([128, 1], F32)
        nb_ring = Ring([P.sb([128, D], BF16) for _ in range(2)])
        tp_ring = Ring([P.ps([128, D], BF16) for _ in range(1)])
        nT = P.sb([128, DC, TS], BF16)
        qkvp = [P.ps([128, 512], F32) for _ in range(6)]
        posi = P.sb([128, 1], I32)
        posf = P.sb([128, 1], F32)
        sn = P.sb([128, 32], F32)
        cs = P.sb([128, 32], F32)
        tm = P.sb([128, 32], F32)
        m_ = P.sb([128, 32], F32)
        sq = P.sb([128, D], F32)
        ssq = P.sb([128, 16], F32)
        qn = P.sb([128, D], F32)
        ra = P.sb([128, 16, 32], F32)
        rb = P.sb([128, 16, 32], F32)
        qr = P.sb([128, D], BF16)
        tpq = P.ps([128, D], BF16)
        qTs = P.sb([128, 8, TS], BF16)
        kTs = P.sb([128, 8, TS], BF16)
        vb = Ring([P.sb([128, D], BF16) for _ in range(2)])
        for t0 in range(0, S, TS):
            norm_supertile(k, c, h, t0, NST, gbc, ring_h, junk, ss, rstd, nb_ring, tp_ring, nT)
            for st in range(NST):
                r0 = t0 + st * 128
                for j in range(6):
                    for dc in range(DC):
                        mm(k, qkvp[j][:], nT[:, dc, st * 128:(st + 1) * 128], wqkv[:, dc, j * 512:(j + 1) * 512],
                           dc == 0, dc == DC - 1, r=[nT, wqkv], w=[qkvp[j]])
                k.dma("sp", lambda e: e.dma_start(out=posi[:], in_=io["positions"][r0:r0 + 128, :]), w=[posi])
                k.op("dve", lambda e: e.tensor_copy(out=posf[:], in_=posi[:]), r=[posi], w=[posf])
                k.op("dve", lambda e: e.tensor_scalar(out=sn[:], in0=inv[:], scalar1=posf[:, 0:1], scalar2=None, op0=ALU.mult),
                     r=[inv, posf], w=[sn])
                k.op("dve", lambda e: e.tensor_copy(out=cs[:], in_=sn[:]), r=[sn], w=[cs])
                range_reduce(k, sn, tm, m_, 32, 0.0)
                range_reduce(k, cs, tm, m_, 32, math.pi / 2)
                k.op("act", lambda e: e.activation(out=sn[:], in_=sn[:], func=AF.Sin), r=[sn], w=[sn])
                k.op("act", lambda e: e.activation(out=cs[:], in_=cs[:], func=AF.Sin), r=[cs], w=[cs])
                csb = cs[:].unsqueeze(1).to_broadcast([128, 16, 32])
                snb = sn[:].unsqueeze(1).to_broadcast([128, 16, 32])
                for which, g_t, dstT in ((0, gq, qTs), (1, gk, kTs)):
                    pA, pB = qkvp[2 * which], qkvp[2 * which + 1]
                    for hf, pp_ in enumerate((pA, pB)):
                        k.op("act", lambda e: e.activation(out=sq[:, hf * 512:(hf + 1) * 512], in_=pp_[:], func=AF.Square),
                             r=[pp_], w=[sq])
                    k.op("dve", lambda e: e.tensor_reduce(out=ssq[:], in_=sq[:].rearrange("p (a d) -> p a d", d=64),
                                                          op=ALU.add, axis=AX.X), r=[sq], w=[ssq])
                    k.op("dve", lambda e: e.tensor_scalar(out=ssq[:], in0=ssq[:], scalar1=1.0 / 64, scalar2=EPS,
                                                          op0=ALU.mult, op1=ALU.add), r=[ssq], w=[ssq])
                    k.op("act", lambda e: e.activation(out=ssq[:], in_=ssq[:], func=AF.Sqrt), r=[ssq], w=[ssq])
                    k.op("dve", lambda e: e.reciprocal(out=ssq[:], in_=ssq[:]), r=[ssq], w=[ssq])
                    for hf, pp_ in enumerate((pA, pB)):
                        k.op("dve", lambda e: e.tensor_tensor(
                            out=qn[:, hf * 512:(hf + 1) * 512].rearrange("p (a d) -> p a d", d=64),
                            in0=pp_[:].rearrange("p (a d) -> p a d", d=64),
                            in1=ssq[:, hf * 8:(hf + 1) * 8].unsqueeze(2).to_broadcast([128, 8, 64]), op=ALU.mult),
                            r=[pp_, ssq], w=[qn])
                    k.op("dve", lambda e: e.tensor_tensor(out=qn[:].rearrange("p (a d) -> p a d", d=64),
                                                          in0=qn[:].rearrange("p (a d) -> p a d", d=64),
                                                          in1=g_t[:].unsqueeze(1).to_broadcast([128, 16, 64]), op=ALU.mult),
                         r=[qn, g_t], w=[qn])
                    q3 = qn[:].rearrange("p (a d) -> p a d", d=64)
                    o3 = qr[:].rearrange("p (a d) -> p a d", d=64)
                    x1, x2 = q3[:, :, 0:32], q3[:, :, 32:64]
                    k.op("dve", lambda e: e.tensor_tensor(out=ra[:], in0=x1, in1=csb, op=ALU.mult), r=[qn, cs], w=[ra])
                    k.op("dve", lambda e: e.tensor_tensor(out=rb[:], in0=x2, in1=snb, op=ALU.mult), r=[qn, sn], w=[rb])
                    k.op("dve", lambda e: e.tensor_tensor(out=o3[:, :, 0:32], in0=ra[:], in1=rb[:], op=ALU.subtract), r=[ra, rb], w=[qr])
                    k.op("dve", lambda e: e.tensor_tensor(out=ra[:], in0=x1, in1=snb, op=ALU.mult), r=[qn, sn], w=[ra])
                    k.op("dve", lambda e: e.tensor_tensor(out=rb[:], in0=x2, in1=csb, op=ALU.mult), r=[qn, cs], w=[rb])
                    k.op("dve", lambda e: e.tensor_tensor(out=o3[:, :, 32:64], in0=ra[:], in1=rb[:], op=ALU.add), r=[ra, rb], w=[qr])
                    for hh in range(8):
                        tr(k, tpq[:, hh * 128:(hh + 1) * 128], qr[:, hh * 128:(hh + 1) * 128], c["idb"][:], r=[qr, c["idb"]], w=[tpq])
                    k.op("act", lambda e: e.activation(out=dstT[:, :, st * 128:(st + 1) * 128],
                                                       in_=tpq[:].rearrange("p (a t) -> p a t", a=8), func=AF.Copy),
                         r=[tpq], w=[dstT])
                v_ = vb.next()
                for hf in range(2):
                    k.op("act", lambda e: e.activation(out=v_[:, hf * 512:(hf + 1) * 512], in_=qkvp[4 + hf][:], func=AF.Copy),
                         r=[qkvp[4 + hf]], w=[v_])
                k.dma("sp", lambda e: e.dma_start(out=io["Vd"][r0:r0 + 128, :], in_=v_[:]), r=[v_], w=[io["d_Vd"]])
            k.dma("sp", lambda e: e.dma_start(out=io["QT"][:, :, t0:t0 + TS].rearrange("a p t -> p a t"), in_=qTs[:]),
                  r=[qTs], w=[io["d_QT"]])
            k.dma("sp", lambda e: e.dma_start(out=io["KT"][:, :, t0:t0 + TS].rearrange("a p t -> p a t"), in_=kTs[:]),
                  r=[kTs], w=[io["d_KT"]])

    with Phase(k, "b2") as P:
        c = make_consts(k, P)
        lv = P.sb([1, 4, 64], F32)
        for i, nm in enumerate(("attn_lam_q1", "attn_lam_k1", "attn_lam_q2", "attn_lam_k2")):
            k.dma("sp", lambda e: e.dma_start(out=lv[:, i, :], in_=io[nm][od:od + 1, :]), w=[lv])
        pr2 = P.sb([1, 2, 64], F32)
        k.op("dve", lambda e: e.tensor_tensor(out=pr2[:, 0, :], in0=lv[:, 0, :], in1=lv[:, 1, :], op=ALU.mult), r=[lv], w=[pr2])
        k.op("dve", lambda e: e.tensor_tensor(out=pr2[:, 1, :], in0=lv[:, 2, :], in1=lv[:, 3, :], op=ALU.mult), r=[lv], w=[pr2])
        s2 = P.sb([1, 2], F32)
        k.op("dve", lambda e: e.tensor_reduce(out=s2[:], in_=pr2[:], op=ALU.add, axis=AX.X), r=[pr2], w=[s2])
        k.op("act", lambda e: e.activation(out=s2[:], in_=s2[:], func=AF.Exp), r=[s2], w=[s2])
        l1 = P.sb([1, 2], F32)
        k.op("dve", lambda e: e.tensor_tensor(out=l1[:, 0:1], in0=s2[:, 1:2], in1=s2[:, 0:1], op=ALU.subtract), r=[s2], w=[l1])
        k.op("dve", lambda e: e.tensor_scalar(out=l1[:, 0:1], in0=l1[:, 0:1], scalar1=-lam_init, scalar2=None, op0=ALU.add),
             r=[l1], w=[l1])
        QB = min(512, S)
        KP = 2 if S >= 256 else 1
        acc_o = [P.ps([128, QB], F32) for _ in range(2)]
        acc_z = [P.ps([128, QB], F32) for _ in range(2)]
        stb = Ring([P.ps([128, KP * QB], F32) for _ in range(2)])
        lp = acc_o[0]
        mm(k, lp[:, 0:1], c["onef"][0:1, 0:128], l1[:, 0:1], True, True, r=[c["onef"], l1], w=[lp])
        nlam = P.sb([128, 1], F32)
        k.op("dve", lambda e: e.tensor_copy(out=nlam[:], in_=lp[:, 0:1]), r=[lp], w=[nlam])
        gs = P.sb([128, 128], F32)
        bcast_row(k, gs, io["attn_subln_g"][od:od + 1, :])
        k.op("dve", lambda e: e.tensor_scalar(out=gs[:], in0=gs[:], scalar1=1.0 - lam_init, scalar2=None, op0=ALU.mult), r=[gs], w=[gs])
        KTs = P.sb([128, S], BF16)
        QTs = P.sb([128, S], BF16)
        Vs = P.sb([128, S // 128, 128], BF16)
        pT = Ring([P.sb([128, KP * QB], BF16) for _ in range(3)])
        r1 = P.sb([128, QB], F32)
        o1 = P.sb([128, QB], F32)
        o2 = P.sb([128, QB], F32)
        ss = P.sb([128, 4], F32)
        junk = P.sb([128, 128], F32)
        att = Ring([P.sb([128, QB // 128, 128], BF16) for _ in range(2)])
        NKT = S // 128
        for hh in range(8):
            k.dma("sp", lambda e: e.dma_start(out=KTs[:], in_=io["KT"][hh]), r=[io["d_KT"]], w=[KTs])
            k.dma("sp", lambda e: e.dma_start(out=QTs[:], in_=io["QT"][hh]), r=[io["d_QT"]], w=[QTs])
            k.dma("sp", lambda e: e.dma_start(out=Vs[:], in_=io["Vd"][:, hh * 128:(hh + 1) * 128].rearrange("(t p) d -> p t d", p=128)),
                  r=[io["d_Vd"]], w=[Vs])
            for q0 in range(0, S, QB):
                for sub in range(2):
                    rows = slice(sub * 64, (sub + 1) * 64)
                    for kp in range(NKT // KP):
                        sb_ = stb.next()
                        for j in range(KP):
                            kt = kp * KP + j
                            mm(k, sb_[:, j * QB:(j + 1) * QB], KTs[rows, kt * 128:(kt + 1) * 128], QTs[rows, q0:q0 + QB], True, True,
                               r=[KTs, QTs], w=[sb_])
                        p_ = pT.next()
                        k.op("act", lambda e: e.activation(out=p_[:], in_=sb_[:], func=AF.Exp), r=[sb_], w=[p_])
                        for j in range(KP):
                            kt = kp * KP + j
                            mm(k, acc_o[sub][:], Vs[:, kt, :], p_[:, j * QB:(j + 1) * QB], kt == 0, kt == NKT - 1,
                               r=[Vs, p_], w=[acc_o[sub]])
                            mm(k, acc_z[sub][:], c["oneb"][:], p_[:, j * QB:(j + 1) * QB], kt == 0, kt == NKT - 1,
                               r=[c["oneb"], p_], w=[acc_z[sub]])
                k.op("dve", lambda e: e.reciprocal(out=r1[:], in_=acc_z[0][:]), r=[acc_z[0]], w=[r1])
                k.op("dve", lambda e: e.tensor_tensor(out=o1[:], in0=acc_o[0][:], in1=r1[:], op=ALU.mult), r=[acc_o[0], r1], w=[o1])
                k.op("dve", lambda e: e.reciprocal(out=r1[:], in_=acc_z[1][:]), r=[acc_z[1]], w=[r1])
                k.op("dve", lambda e: e.tensor_tensor(out=o2[:], in0=acc_o[1][:], in1=r1[:], op=ALU.mult), r=[acc_o[1], r1], w=[o2])
                k.op("dve", lambda e: e.scalar_tensor_tensor(out=o1[:], in0=o2[:], scalar=nlam[:, 0:1], in1=o1[:],
                                                             op0=ALU.mult, op1=ALU.add), r=[o2, nlam, o1], w=[o1])
                tq = acc_z[1]
                a_ = att.next()
                for qt in range(QB // 128):
                    tr(k, tq[:, qt * 128:(qt + 1) * 128], o1[:, qt * 128:(qt + 1) * 128], c["idf"][:], r=[o1, c["idf"]], w=[tq])
                for qt in range(QB // 128):
                    k.op("act", lambda e: e.activation(out=junk[:], in_=tq[:, qt * 128:(qt + 1) * 128], func=AF.Square,
                                                       accum_out=ss[:, qt:qt + 1]), r=[tq], w=[junk, ss])
                k.op("dve", lambda e: e.tensor_scalar(out=ss[:], in0=ss[:], scalar1=1.0 / 128, scalar2=EPS, op0=ALU.mult, op1=ALU.add),
                     r=[ss], w=[ss])
                k.op("act", lambda e: e.activation(out=ss[:], in_=ss[:], func=AF.Sqrt), r=[ss], w=[ss])
                k.op("dve", lambda e: e.reciprocal(out=ss[:], in_=ss[:]), r=[ss], w=[ss])
                for qt in range(QB // 128):
                    k.op("dve", lambda e: e.scalar_tensor_tensor(out=a_[:, qt, :], in0=tq[:, qt * 128:(qt + 1) * 128],
                                                                 scalar=ss[:, qt:qt + 1], in1=gs[:], op0=ALU.mult, op1=ALU.mult),
                         r=[tq, ss, gs], w=[a_])
                k.dma("sp", lambda e: e.dma_start(
                    out=io["AO"][q0:q0 + QB, hh * 128:(hh + 1) * 128].rearrange("(t p) d -> p t d", p=128), in_=a_[:]),
                    r=[a_], w=[io["d_AO"]])

    with Phase(k, "b3") as P:
        c = make_consts(k, P)
        wo = P.sb([128, DC, D], BF16)
        load_w_bf16(k, wo, io["attn_w_out"][od], DC)
        ao = Ring([P.sb([128, D], BF16) for _ in range(2)])
        tp = Ring([P.ps([128, D], BF16) for _ in range(2)])
        aT = Ring([P.sb([128, DC, 128], BF16) for _ in range(2)])
        mp_r = Ring([P.ps([128, 512], F32) for _ in range(2)])
        ht_r = Ring([P.sb([128, D], F32) for _ in range(2)])
        for t0 in range(0, S, 128):
            a_ = ao.next()
            k.dma("sp", lambda e: e.dma_start(out=a_[:], in_=io["AO"][t0:t0 + 128, :]), r=[io["d_AO"]], w=[a_])
            t_ = tp.next()
            for dc in range(DC):
                tr(k, t_[:, dc * 128:(dc + 1) * 128], a_[:, dc * 128:(dc + 1) * 128], c["idb"][:], r=[a_, c["idb"]], w=[t_])
            at = aT.next()
            k.op("act", lambda e: e.activation(out=at[:], in_=t_[:].rearrange("p (c t) -> p c t", c=DC), func=AF.Copy), r=[t_], w=[at])
            ht = ht_r.next()
            k.dma("sp", lambda e: e.dma_start(out=ht[:], in_=h[t0:t0 + 128, :]), r=[io["d_h"]], w=[ht])
            for hf in range(2):
                mp = mp_r.next()
                for dc in range(DC):
                    mm(k, mp[:], at[:, dc, :], wo[:, dc, hf * 512:(hf + 1) * 512], dc == 0, dc == DC - 1, r=[at, wo], w=[mp])
                k.op("dve", lambda e: e.tensor_tensor(out=ht[:, hf * 512:(hf + 1) * 512], in0=mp[:],
                                                      in1=ht[:, hf * 512:(hf + 1) * 512], op=ALU.add), r=[mp, ht], w=[ht])
            k.dma("sp", lambda e: e.dma_start(out=h[t0:t0 + 128, :], in_=ht[:]), r=[ht], w=[io["d_h"]])


INPUT_SHAPES = None


def input_shapes(cfg):
    S, E, FF, DEPTH, NE, NO = cfg.S, cfg.E, cfg.FF, cfg.DEPTH, cfg.NE, cfg.NO
    sh = {
        "x": ([S, D], F32), "positions": ([S, 1], I32),
        "norm_mix_g": ([DEPTH, D], F32), "norm_ffn_g": ([DEPTH, D], F32),
        "hyb_w_in": ([NE, D, D], F32), "hyb_w_out": ([NE, D, D], F32),
        "s5_lam_re": ([NE, 2, 32, 64], F32), "s5_lam_im": ([NE, 2, 32, 64], F32), "s5_log_dt": ([NE, 2, 32], F32),
        "s5_b_re": ([NE, 2, 32, 64, 16], F32), "s5_b_im": ([NE, 2, 32, 64, 16], F32),
        "s5_c_re": ([NE, 2, 32, 16, 64], F32), "s5_c_im": ([NE, 2, 32, 16, 64], F32),
        "s5_d": ([NE, 512], F32), "s5_w_glu": ([NE, 512, 512], F32), "s5_b_glu": ([NE, 512], F32),
        "pool_w": ([NE, 4, 128, 128], F32), "pool_scale": ([NE, 512], F32),
        "moe_w_router": ([DEPTH, D, E], F32), "moe_w_gate": ([DEPTH, E, D, FF], F32),
        "moe_w_up": ([DEPTH, E, D, FF], F32), "moe_w_down": ([DEPTH, E, FF, D], F32),
    }
    if NO > 0:
        sh.update({
            "attn_w_qkv": ([NO, D, 3 * D], F32), "attn_w_out": ([NO, D, D], F32),
            "attn_q_norm_g": ([NO, 64], F32), "attn_k_norm_g": ([NO, 64], F32),
            "attn_lam_q1": ([NO, 64], F32), "attn_lam_k1": ([NO, 64], F32),
            "attn_lam_q2": ([NO, 64], F32), "attn_lam_k2": ([NO, 64], F32), "attn_subln_g": ([NO, 128], F32),
        })
    return sh


def build(cfg, phases=None):
    k = KB()
    nc = k.nc
    io = {}
    for nm, (shape, dt) in input_shapes(cfg).items():
        io[nm] = nc.dram_tensor(nm, shape, dt, kind="ExternalInput").ap()
    S, E = cfg.S, cfg.E
    io["out"] = nc.dram_tensor("out", [S, D], F32, kind="ExternalOutput").ap()
    scratch = {"uT": ([512, S], BF16), "zpT": ([512, S], F32), "yT": ([512, S], F32), "hn": ([S, D], BF16),
               "affd": ([S, E], F32), "csd": ([E, S], F32), "idxd": ([128, E * (cfg.CAP // 128)], I32),
               "QT": ([8, 128, S], BF16), "KT": ([8, 128, S], BF16), "Vd": ([S, D], BF16), "AO": ([S, D], BF16)}
    for nm, (shape, dt) in scratch.items():
        io[nm] = nc.dram_tensor(nm, shape, dt, kind="Internal").ap()
        io["d_" + nm] = Dep()
    io["d_h"] = Dep()
    with nc.allow_non_contiguous_dma(reason="small strided parameter loads"):
        _emit(k, cfg, io, phases)
    k.barrier()
    return k


def _emit(k, cfg, io, phases):
    phase_copy_in(k, cfg, io["x"], io["out"])
    for layer in range(cfg.DEPTH):
        if layer % 2 == 0:
            if phases is None or "mix" in phases:
                phase_even_in(k, cfg, io, layer)
                phase_s5(k, cfg, io, layer)
                phase_even_out(k, cfg, io, layer)
        else:
            if phases is None or "mix" in phases:
                phase_attn(k, cfg, io, layer)
        if phases is None or "moe" in phases:
            phase_moe(k, cfg, io, layer)


_CACHE = {}


def kernel(**inputs):
    cfg = Cfg()
    if "k" not in _CACHE:
        _CACHE["k"] = build(cfg)
    k = _CACHE["k"]
    B = inputs["x"].shape[0]
    names = list(input_shapes(cfg).keys())
    in_maps = []
    for b in range(B):
        m = {}
        for nm in names:
            a = np.asarray(inputs[nm])
            if nm == "x":
                a = np.ascontiguousarray(a[b])
            elif nm == "positions":
                a = np.ascontiguousarray(a[b].reshape(cfg.S, 1).astype(np.int32))
            m[nm] = a
        in_maps.append(m)
    res = run_bass_kernel_spmd(k.nc, in_maps, core_ids=list(range(B)))
    return np.stack([np.asarray(r["out"]) for r in res.results], axis=0).astype(np.float32)
```

```python
import math
from contextlib import ExitStack
import numpy as np
import concourse.bass as bass
import concourse.mybir as mybir
from concourse.bass_utils import run_bass_kernel_spmd

F32 = mybir.dt.float32
BF16 = mybir.dt.bfloat16
I32 = mybir.dt.int32
U8 = mybir.dt.uint8
AF = mybir.ActivationFunctionType
ALU = mybir.AluOpType
AX = mybir.AxisListType

D = 1024
DC = 8
EPS = 1e-6
TWO_PI = 2.0 * math.pi
CW1 = 6.28125
CW2 = TWO_PI - CW1
MAGIC = 12582912.0


class Dep:
    __slots__ = ("w", "r")

    def __init__(self):
        self.w = None
        self.r = {}


class T:
    def __init__(self, t):
        self.t = t
        self.d = Dep()

    def __getitem__(self, key):
        return self.t[key]


class KB:
    NDMA = 8

    def __init__(self):
        self.nc = bass.Bass("TRN2", target_bir_lowering=False)
        nc = self.nc
        self.eng = {"pe": nc.tensor, "dve": nc.vector, "act": nc.scalar, "pool": nc.gpsimd, "sp": nc.sync}
        self.sems = {}
        self.cnt = {}
        for e in ("pe", "dve", "act", "pool"):
            self.sems[e] = nc.alloc_semaphore("c_" + e)
            self.cnt[e] = 0
        self.dma_pool = {}
        for q in ("sp", "act", "pool"):
            lst = []
            for i in range(self.NDMA):
                nm = "d_%s%d" % (q, i)
                self.sems[nm] = nc.alloc_semaphore(nm)
                self.cnt[nm] = 0
                lst.append(nm)
            self.dma_pool[q] = [lst, 0]
        self.seen = {e: {} for e in self.eng}
        self.ninst = 0

    def _wait(self, e, tok):
        if tok is None:
            return
        s, v = tok
        if e == "pe" and s == "pe":
            return
        if self.seen[e].get(s, 0) >= v:
            return
        self.eng[e].wait_ge(self.sems[s], v)
        self.seen[e][s] = v

    def _pre(self, e, r, w):
        for d in r:
            self._wait(e, d.w)
        for d in w:
            self._wait(e, d.w)
            for s, v in d.r.items():
                self._wait(e, (s, v))

    def _post(self, tok, r, w):
        s, v = tok
        for d in r:
            if d.r.get(s, 0) < v:
                d.r[s] = v
        for d in w:
            d.w = tok
            d.r = {}

    @staticmethod
    def _deps(lst):
        return [x.d if isinstance(x, T) else x for x in lst]

    def op(self, e, fn, r=(), w=()):
        r = self._deps(r)
        w = self._deps(w)
        self._pre(e, r, w)
        inst = fn(self.eng[e])
        self.cnt[e] += 1
        inst.then_inc(self.sems[e], 1)
        self._post((e, self.cnt[e]), r, w)
        self.ninst += 1

    def dma(self, q, fn, r=(), w=()):
        r = self._deps(r)
        w = self._deps(w)
        lst, idx = self.dma_pool[q]
        nm = lst[idx % len(lst)]
        self.dma_pool[q][1] = idx + 1
        if self.cnt[nm]:
            self._wait(q, (nm, self.cnt[nm]))
        self._pre(q, r, w)
        inst = fn(self.eng[q])
        self.cnt[nm] += 16
        inst.then_inc(self.sems[nm], 16)
        self._post((nm, self.cnt[nm]), r, w)
        self.ninst += 1

    def barrier(self, engines=("pe", "dve", "act", "pool", "sp")):
        for e in engines:
            for s, v in self.cnt.items():
                if v:
                    self._wait(e, (s, v))


class Phase:
    def __init__(self, k, name):
        self.k = k
        self.name = name
        self.es = ExitStack()
        self.i = 0

    def __enter__(self):
        self.es.__enter__()
        return self

    def __exit__(self, *a):
        self.k.barrier()
        return self.es.__exit__(*a)

    def sb(self, shape, dt, name=None):
        self.k.ninst_names = getattr(self.k, "ninst_names", 0) + 1
        nm = "%s_%s%d" % (self.name, name or "s", self.k.ninst_names)
        return T(self.es.enter_context(self.k.nc.sbuf_tensor(nm, list(shape), dt)))

    def ps(self, shape, dt, name=None):
        self.k.ninst_names = getattr(self.k, "ninst_names", 0) + 1
        nm = "%s_%s%d" % (self.name, name or "p", self.k.ninst_names)
        return T(self.es.enter_context(self.k.nc.psum_tensor(nm, list(shape), dt)))


class Ring:
    def __init__(self, items):
        self.items = items
        self.i = 0

    def next(self):
        x = self.items[self.i % len(self.items)]
        self.i += 1
        return x


class Cfg:
    def __init__(self, S=16384, DEPTH=4, E=16, FF=2048):
        self.S = S
        self.DEPTH = DEPTH
        self.E = E
        self.FF = FF
        self.CAP = 2 * S // E
        self.NE = (DEPTH + 1) // 2
        self.NO = DEPTH // 2


def mm(k, out, lhsT, rhs, start, stop, r, w):
    k.op("pe", lambda e: e.matmul(out, lhsT, rhs, start=start, stop=stop), r=r, w=w)


def tr(k, out, in_, ident, r, w):
    k.op("pe", lambda e: e.transpose(out, in_, ident), r=r, w=w)


def make_consts(k, P):
    c = {}
    io = P.sb([128, 128], F32, "iota")
    k.op("pool", lambda e: e.iota(io[:], pattern=[[1, 128]], base=0, channel_multiplier=-1,
                                  allow_small_or_imprecise_dtypes=True), w=[io])
    c["idf"] = P.sb([128, 128], F32, "idf")
    k.op("dve", lambda e: e.tensor_scalar(out=c["idf"][:], in0=io[:], scalar1=0.0, scalar2=None,
                                          op0=ALU.is_equal), r=[io], w=[c["idf"]])
    c["idb"] = P.sb([128, 128], BF16, "idb")
    k.op("dve", lambda e: e.tensor_copy(out=c["idb"][:], in_=c["idf"][:]), r=[c["idf"]], w=[c["idb"]])
    c["oneb"] = P.sb([128, 128], BF16, "oneb")
    k.op("dve", lambda e: e.memset(c["oneb"][:], 1.0), w=[c["oneb"]])
    c["onef"] = P.sb([128, 512], F32, "onef")
    k.op("dve", lambda e: e.memset(c["onef"][:], 1.0), w=[c["onef"]])
    return c


def rms_rstd(k, x_ap, xdep, junk, ss, rstd, n, eps=EPS):
    k.op("act", lambda e: e.activation(out=junk[:, 0:n], in_=x_ap, func=AF.Square, accum_out=ss[:, 0:1]),
         r=[xdep], w=[junk, ss])
    k.op("dve", lambda e: e.tensor_scalar(out=ss[:, 0:1], in0=ss[:, 0:1], scalar1=1.0 / n, scalar2=eps,
                                          op0=ALU.mult, op1=ALU.add), r=[ss], w=[ss])
    k.op("act", lambda e: e.activation(out=ss[:, 0:1], in_=ss[:, 0:1], func=AF.Sqrt), r=[ss], w=[ss])
    k.op("dve", lambda e: e.reciprocal(out=rstd[:, 0:1], in_=ss[:, 0:1]), r=[ss], w=[rstd])


def load_w_bf16(k, dst, src_ap, rows_chunks):
    for c in range(rows_chunks):
        k.dma("pool", lambda e: e.dma_start(out=dst[:, c, :], in_=src_ap[c * 128:(c + 1) * 128, :]), w=[dst])


def norm_supertile(k, c, h_ap, t0, nsub, gbc, ring_h, junk, ss, rstd, nb_ring, tp_ring, nT):
    for st in range(nsub):
        ht = ring_h.next()
        r0 = t0 + st * 128
        k.dma("sp", lambda e: e.dma_start(out=ht[:], in_=h_ap[r0:r0 + 128, :]), w=[ht])
        rms_rstd(k, ht[:], ht, junk, ss, rstd, D)
        nb = nb_ring.next()
        k.op("dve", lambda e: e.scalar_tensor_tensor(out=nb[:], in0=ht[:], scalar=rstd[:, 0:1], in1=gbc[:],
                                                     op0=ALU.mult, op1=ALU.mult), r=[ht, rstd, gbc], w=[nb])
        tp = tp_ring.next()
        for dc in range(DC):
            tr(k, tp[:, dc * 128:(dc + 1) * 128], nb[:, dc * 128:(dc + 1) * 128], c["idb"][:], r=[nb, c["idb"]], w=[tp])
        k.op("act", lambda e: e.activation(out=nT[:, :, st * 128:(st + 1) * 128],
                                           in_=tp[:].rearrange("p (c t) -> p c t", c=DC), func=AF.Copy),
             r=[tp], w=[nT])


def bcast_row(k, dst, src_row_ap):
    k.dma("sp", lambda e: e.dma_start(out=dst[:], in_=src_row_ap.to_broadcast([128, src_row_ap.shape[-1]])), w=[dst])


def phase_copy_in(k, cfg, x, out):
    S = cfg.S
    with Phase(k, "cp") as P:
        ring = Ring([P.sb([128, 8192], F32) for _ in range(2)])
        xv = x.rearrange("(p r) d -> p (r d)", p=128)
        ov = out.rearrange("(p r) d -> p (r d)", p=128)
        tot = S // 128 * D
        for o in range(0, tot, 8192):
            n = min(8192, tot - o)
            b = ring.next()
            k.dma("sp", lambda e: e.dma_start(out=b[:, 0:n], in_=xv[:, o:o + n]), w=[b])
            k.dma("sp", lambda e: e.dma_start(out=ov[:, o:o + n], in_=b[:, 0:n]), r=[b])


def phase_even_in(k, cfg, io, layer):
    S = cfg.S
    ev = layer // 2
    h = io["out"]
    with Phase(k, "a1") as P:
        c = make_consts(k, P)
        gbc = P.sb([128, D], F32)
        bcast_row(k, gbc, io["norm_mix_g"][layer:layer + 1, :])
        win = P.sb([128, DC, D], BF16)
        load_w_bf16(k, win, io["hyb_w_in"][ev], DC)
        ring_h = Ring([P.sb([128, D], F32) for _ in range(2)])
        junk = P.sb([128, D], F32)
        ss = P.sb([128, 1], F32)
        rstd = P.sb([128, 1], F32)
        nb_ring = Ring([P.sb([128, D], BF16) for _ in range(2)])
        tp_ring = Ring([P.ps([128, D], BF16) for _ in range(2)])
        TS = min(512, S)
        nT_ring = Ring([P.sb([128, DC, TS], BF16) for _ in range(2)])
        zp_ring = Ring([P.ps([128, TS], F32) for _ in range(3)])
        ub_ring = Ring([P.sb([128, TS], BF16) for _ in range(2)])
        zf_ring = Ring([P.sb([128, TS], F32) for _ in range(2)])
        for t0 in range(0, S, TS):
            nT = nT_ring.next()
            norm_supertile(k, c, h, t0, TS // 128, gbc, ring_h, junk, ss, rstd, nb_ring, tp_ring, nT)
            for cc in range(8):
                zp = zp_ring.next()
                for dc in range(DC):
                    mm(k, zp[:], win[:, dc, cc * 128:(cc + 1) * 128], nT[:, dc, :], dc == 0, dc == DC - 1,
                       r=[win, nT], w=[zp])
                if cc < 4:
                    ub = ub_ring.next()
                    k.op("act", lambda e: e.activation(out=ub[:], in_=zp[:], func=AF.Copy), r=[zp], w=[ub])
                    k.dma("sp", lambda e: e.dma_start(out=io["uT"][cc * 128:(cc + 1) * 128, t0:t0 + TS], in_=ub[:]),
                          r=[ub], w=[io["d_uT"]])
                else:
                    zf = zf_ring.next()
                    k.op("dve", lambda e: e.tensor_copy(out=zf[:], in_=zp[:]), r=[zp], w=[zf])
                    k.dma("sp", lambda e: e.dma_start(out=io["zpT"][(cc - 4) * 128:(cc - 3) * 128, t0:t0 + TS], in_=zf[:]),
                          r=[zf], w=[io["d_zpT"]])


def range_reduce(k, ang, tmp, m, n, shift=0.0):
    if shift != 0.0:
        k.op("dve", lambda e: e.tensor_scalar(out=ang[:, 0:n], in0=ang[:, 0:n], scalar1=shift, scalar2=None, op0=ALU.add),
             r=[ang], w=[ang])
    k.op("dve", lambda e: e.tensor_scalar(out=tmp[:, 0:n], in0=ang[:, 0:n], scalar1=1.0 / TWO_PI, scalar2=MAGIC,
                                          op0=ALU.mult, op1=ALU.add), r=[ang], w=[tmp])
    k.op("dve", lambda e: e.tensor_scalar(out=m[:, 0:n], in0=tmp[:, 0:n], scalar1=MAGIC, scalar2=None, op0=ALU.subtract),
         r=[tmp], w=[m])
    k.op("dve", lambda e: e.scalar_tensor_tensor(out=tmp[:, 0:n], in0=m[:, 0:n], scalar=-CW1, in1=ang[:, 0:n],
                                                 op0=ALU.mult, op1=ALU.add), r=[m, ang], w=[tmp])
    k.op("dve", lambda e: e.scalar_tensor_tensor(out=ang[:, 0:n], in0=m[:, 0:n], scalar=-CW2, in1=tmp[:, 0:n],
                                                 op0=ALU.mult, op1=ALU.add), r=[m, tmp], w=[ang])
    k.op("dve", lambda e: e.tensor_scalar(out=ang[:, 0:n], in0=ang[:, 0:n], scalar1=3.1415925, scalar2=-3.1415925,
                                          op0=ALU.min, op1=ALU.max), r=[ang], w=[ang])


def phase_s5(k, cfg, io, layer):
    S = cfg.S
    ev = layer // 2
    TB = min(512, S)
    NB = S // TB
    with Phase(k, "s5") as P:
        c = make_consts(k, P)
        iot = P.sb([128, TB], F32)
        k.op("pool", lambda e: e.iota(iot[:], pattern=[[1, TB]], base=1, channel_multiplier=0,
                                      allow_small_or_imprecise_dtypes=True), w=[iot])
        cosT = [P.sb([128, TB], F32) for _ in range(4)]
        sinT = [P.sb([128, TB], F32) for _ in range(4)]
        Ar = [P.sb([128, TB], F32) for _ in range(4)]
        Wre = [P.sb([128, 128], BF16) for _ in range(4)]
        Wim = [P.sb([128, 128], BF16) for _ in range(4)]
        Cre = [P.sb([128, 128], F32) for _ in range(4)]
        Cim = [P.sb([128, 128], F32) for _ in range(4)]
        car_re = [P.sb([128, 1], F32) for _ in range(4)]
        car_im = [P.sb([128, 1], F32) for _ in range(4)]
        dsk = P.sb([128, 128], BF16)
        pr = {n: P.sb([128, 1], F32, n) for n in ("lre", "lim", "ldt", "dt", "th", "rho", "r", "are", "aim", "am1",
                                                  "den", "kre", "kim", "t1", "t2", "tm", "mm", "dcol")}
        bre = P.sb([128, 16], F32)
        bim = P.sb([128, 16], F32)
        bbr = P.sb([128, 16], F32)
        bbi = P.sb([128, 16], F32)
        tq = P.sb([128, 16], F32)
        BW = P.sb([128, 128], F32)
        Cn = P.sb([32, 128], F32)
        tmpT = P.sb([128, TB], F32)
        mT = P.sb([128, TB], F32)
        pp = P.ps([128, 128], F32)
        ub_ring = Ring([P.sb([128, TB], BF16) for _ in range(2)])
        bu_re = Ring([P.ps([128, TB], F32) for _ in range(2)])
        bu_im = Ring([P.ps([128, TB], F32) for _ in range(2)])
        yps_ring = Ring([P.ps([128, TB], F32) for _ in range(2)])
        t1 = Ring([P.sb([128, TB], F32) for _ in range(8)])
        t2 = Ring([P.sb([128, TB], F32) for _ in range(8)])
        t3 = Ring([P.sb([128, TB], F32) for _ in range(8)])
        t4 = Ring([P.sb([128, TB], F32) for _ in range(8)])
        xre = Ring([P.sb([128, TB], F32) for _ in range(4)])
        xim = Ring([P.sb([128, TB], F32) for _ in range(4)])
        gre = Ring([P.sb([128, TB], F32) for _ in range(4)])
        gim = Ring([P.sb([128, TB], F32) for _ in range(4)])
        hre = Ring([P.sb([128, TB], F32) for _ in range(4)])
        him = Ring([P.sb([128, TB], F32) for _ in range(4)])
        ysb = Ring([P.sb([128, TB], F32) for _ in range(2)])
        yold = Ring([P.sb([128, TB], F32) for _ in range(2)])

        def col(ap3, dirn, g0):
            return ap3[ev, dirn, g0:g0 + 2, :].rearrange("g (s o) -> (g s) o", o=1)

        def prep_tile(dirn, j, g0, r0):
            k.dma("sp", lambda e: e.dma_start(out=pr["lre"][:], in_=col(io["s5_lam_re"], dirn, g0)), w=[pr["lre"]])
            k.dma("sp", lambda e: e.dma_start(out=pr["lim"][:], in_=col(io["s5_lam_im"], dirn, g0)), w=[pr["lim"]])
            for gl in range(2):
                src = io["s5_log_dt"][ev, dirn:dirn + 1, g0 + gl:g0 + gl + 1]
                k.dma("sp", lambda e: e.dma_start(out=pr["ldt"][gl * 64:(gl + 1) * 64, :], in_=src.to_broadcast([64, 1])),
                      w=[pr["ldt"]])
            k.op("act", lambda e: e.activation(out=pr["dt"][:], in_=pr["ldt"][:], func=AF.Exp), r=[pr["ldt"]], w=[pr["dt"]])
            k.op("dve", lambda e: e.tensor_tensor(out=pr["th"][:], in0=pr["lim"][:], in1=pr["dt"][:], op=ALU.mult),
                 r=[pr["lim"], pr["dt"]], w=[pr["th"]])
            k.op("dve", lambda e: e.tensor_tensor(out=pr["rho"][:], in0=pr["lre"][:], in1=pr["dt"][:], op=ALU.mult),
                 r=[pr["lre"], pr["dt"]], w=[pr["rho"]])
            k.op("act", lambda e: e.activation(out=pr["r"][:], in_=pr["rho"][:], func=AF.Exp), r=[pr["rho"]], w=[pr["r"]])
            k.op("dve", lambda e: e.tensor_scalar(out=sinT[j][:], in0=iot[:], scalar1=pr["th"][:, 0:1], scalar2=None,
                                                  op0=ALU.mult), r=[iot, pr["th"]], w=[sinT[j]])
            k.op("dve", lambda e: e.tensor_copy(out=cosT[j][:], in_=sinT[j][:]), r=[sinT[j]], w=[cosT[j]])
            range_reduce(k, sinT[j], tmpT, mT, TB, 0.0)
            range_reduce(k, cosT[j], tmpT, mT, TB, math.pi / 2)
            k.op("act", lambda e: e.activation(out=sinT[j][:], in_=sinT[j][:], func=AF.Sin), r=[sinT[j]], w=[sinT[j]])
            k.op("act", lambda e: e.activation(out=cosT[j][:], in_=cosT[j][:], func=AF.Sin), r=[cosT[j]], w=[cosT[j]])
            k.op("dve", lambda e: e.tensor_scalar(out=Ar[j][:], in0=c["onef"][:, 0:TB], scalar1=pr["r"][:, 0:1], scalar2=None,
                                                  op0=ALU.mult), r=[c["onef"], pr["r"]], w=[Ar[j]])
            k.op("dve", lambda e: e.tensor_tensor(out=pr["are"][:], in0=pr["r"][:], in1=cosT[j][:, 0:1], op=ALU.mult),
                 r=[pr["r"], cosT[j]], w=[pr["are"]])
            k.op("dve", lambda e: e.tensor_tensor(out=pr["aim"][:], in0=pr["r"][:], in1=sinT[j][:, 0:1], op=ALU.mult),
                 r=[pr["r"], sinT[j]], w=[pr["aim"]])
            k.op("dve", lambda e: e.tensor_scalar(out=pr["am1"][:], in0=pr["are"][:], scalar1=-1.0, scalar2=None, op0=ALU.add),
                 r=[pr["are"]], w=[pr["am1"]])
            k.op("dve", lambda e: e.tensor_tensor(out=pr["t1"][:], in0=pr["lre"][:], in1=pr["lre"][:], op=ALU.mult),
                 r=[pr["lre"]], w=[pr["t1"]])
            k.op("dve", lambda e: e.scalar_tensor_tensor(out=pr["t2"][:], in0=pr["lim"][:], scalar=pr["lim"][:, 0:1],
                                                         in1=pr["t1"][:], op0=ALU.mult, op1=ALU.add),
                 r=[pr["lim"], pr["t1"]], w=[pr["t2"]])
            k.op("dve", lambda e: e.reciprocal(out=pr["den"][:], in_=pr["t2"][:]), r=[pr["t2"]], w=[pr["den"]])
            k.op("dve", lambda e: e.tensor_tensor(out=pr["t1"][:], in0=pr["am1"][:], in1=pr["lre"][:], op=ALU.mult),
                 r=[pr["am1"], pr["lre"]], w=[pr["t1"]])
            k.op("dve", lambda e: e.scalar_tensor_tensor(out=pr["t2"][:], in0=pr["aim"][:], scalar=pr["lim"][:, 0:1],
                                                         in1=pr["t1"][:], op0=ALU.mult, op1=ALU.add),
                 r=[pr["aim"], pr["lim"], pr["t1"]], w=[pr["t2"]])
            k.op("dve", lambda e: e.tensor_tensor(out=pr["kre"][:], in0=pr["t2"][:], in1=pr["den"][:], op=ALU.mult),
                 r=[pr["t2"], pr["den"]], w=[pr["kre"]])
            k.op("dve", lambda e: e.tensor_tensor(out=pr["t1"][:], in0=pr["am1"][:], in1=pr["lim"][:], op=ALU.mult),
                 r=[pr["am1"], pr["lim"]], w=[pr["t1"]])
            k.op("dve", lambda e: e.scalar_tensor_tensor(out=pr["t2"][:], in0=pr["aim"][:], scalar=pr["lre"][:, 0:1],
                                                         in1=pr["t1"][:], op0=ALU.mult, op1=ALU.subtract),
                 r=[pr["aim"], pr["lre"], pr["t1"]], w=[pr["t2"]])
            k.op("dve", lambda e: e.tensor_tensor(out=pr["kim"][:], in0=pr["t2"][:], in1=pr["den"][:], op=ALU.mult),
                 r=[pr["t2"], pr["den"]], w=[pr["kim"]])
            k.dma("sp", lambda e: e.dma_start(out=bre[:], in_=io["s5_b_re"][ev, dirn, g0:g0 + 2].rearrange("g s c -> (g s) c")),
                  w=[bre])
            k.dma("sp", lambda e: e.dma_start(out=bim[:], in_=io["s5_b_im"][ev, dirn, g0:g0 + 2].rearrange("g s c -> (g s) c")),
                  w=[bim])
            k.op("dve", lambda e: e.tensor_scalar(out=tq[:], in0=bim[:], scalar1=pr["kim"][:, 0:1], scalar2=None, op0=ALU.mult),
                 r=[bim, pr["kim"]], w=[tq])
            k.op("dve", lambda e: e.scalar_tensor_tensor(out=bbr[:], in0=bre[:], scalar=pr["kre"][:, 0:1], in1=tq[:],
                                                         op0=ALU.mult, op1=ALU.subtract), r=[bre, pr["kre"], tq], w=[bbr])
            k.op("dve", lambda e: e.tensor_scalar(out=tq[:], in0=bre[:], scalar1=pr["kim"][:, 0:1], scalar2=None, op0=ALU.mult),
                 r=[bre, pr["kim"]], w=[tq])
            k.op("dve", lambda e: e.scalar_tensor_tensor(out=bbi[:], in0=bim[:], scalar=pr["kre"][:, 0:1], in1=tq[:],
                                                         op0=ALU.mult, op1=ALU.add), r=[bim, pr["kre"], tq], w=[bbi])
            for src, dst in ((bbr, Wre[j]), (bbi, Wim[j])):
                k.op("dve", lambda e: e.memset(BW[:], 0.0), w=[BW])
                for gl in range(2):
                    k.op("dve", lambda e: e.tensor_copy(out=BW[gl * 64:(gl + 1) * 64, r0 + gl * 16:r0 + gl * 16 + 16],
                                                        in_=src[gl * 64:(gl + 1) * 64, :]), r=[src], w=[BW])
                tr(k, pp[:], BW[:], c["idf"][:], r=[BW, c["idf"]], w=[pp])
                k.op("dve", lambda e: e.tensor_copy(out=dst[:], in_=pp[:]), r=[pp], w=[dst])
            for src_name, dst, sgn in (("s5_c_re", Cre[j], 1.0), ("s5_c_im", Cim[j], -1.0)):
                k.op("dve", lambda e: e.memset(Cn[:], 0.0), w=[Cn])
                for gl in range(2):
                    k.dma("sp", lambda e: e.dma_start(out=Cn[gl * 16:(gl + 1) * 16, gl * 64:(gl + 1) * 64],
                                                      in_=io[src_name][ev, dirn, g0 + gl]), w=[Cn])
                tr(k, pp[:, 0:32], Cn[:], c["idf"][0:32, 0:32], r=[Cn, c["idf"]], w=[pp])
                k.op("dve", lambda e: e.memset(dst[:], 0.0), w=[dst])
                k.op("dve", lambda e: e.tensor_scalar(out=dst[:, r0:r0 + 32], in0=pp[:, 0:32], scalar1=sgn, scalar2=None,
                                                      op0=ALU.mult), r=[pp], w=[dst])
            k.op("dve", lambda e: e.memset(car_re[j][:], 0.0), w=[car_re[j]])
            k.op("dve", lambda e: e.memset(car_im[j][:], 0.0), w=[car_im[j]])

        for dirn in range(2):
            rev = dirn == 1

            def V(ap):
                return ap[:, ::-1] if rev else ap
            for cc in range(4):
                for j in range(4):
                    prep_tile(dirn, j, cc * 8 + 2 * j, 32 * j)
                if not rev:
                    k.dma("sp", lambda e: e.dma_start(out=pr["dcol"][:],
                                                      in_=io["s5_d"][ev:ev + 1, cc * 128:(cc + 1) * 128].rearrange("o c -> c o")),
                          w=[pr["dcol"]])
                    k.op("dve", lambda e: e.tensor_scalar(out=dsk[:], in0=c["idf"][:], scalar1=pr["dcol"][:, 0:1], scalar2=None,
                                                          op0=ALU.mult), r=[c["idf"], pr["dcol"]], w=[dsk])
                blks = list(range(NB))
                if rev:
                    blks = blks[::-1]
                for b in blks:
                    ub = ub_ring.next()
                    k.dma("sp", lambda e: e.dma_start(out=ub[:], in_=io["uT"][cc * 128:(cc + 1) * 128, b * TB:(b + 1) * TB]),
                          r=[io["d_uT"]], w=[ub])
                    yps = yps_ring.next()
                    st1 = []
                    for j in range(4):
                        pre, pim = bu_re.next(), bu_im.next()
                        mm(k, pre[:], Wre[j][:], ub[:], True, True, r=[Wre[j], ub], w=[pre])
                        mm(k, pim[:], Wim[j][:], ub[:], True, True, r=[Wim[j], ub], w=[pim])
                        a1, a2, a3, a4 = t1.next(), t2.next(), t3.next(), t4.next()
                        cs, sn = V(cosT[j][:]), V(sinT[j][:])
                        k.op("dve", lambda e: e.tensor_tensor(out=a1[:], in0=pre[:], in1=cs, op=ALU.mult), r=[pre, cosT[j]], w=[a1])
                        k.op("dve", lambda e: e.tensor_tensor(out=a2[:], in0=pim[:], in1=sn, op=ALU.mult), r=[pim, sinT[j]], w=[a2])
                        k.op("dve", lambda e: e.tensor_tensor(out=a3[:], in0=pim[:], in1=cs, op=ALU.mult), r=[pim, cosT[j]], w=[a3])
                        k.op("dve", lambda e: e.tensor_tensor(out=a4[:], in0=pre[:], in1=sn, op=ALU.mult), r=[pre, sinT[j]], w=[a4])
                        st1.append((a1, a2, a3, a4, cs, sn))
                    st2 = []
                    for j in range(4):
                        a1, a2, a3, a4, cs, sn = st1[j]
                        xr, xi = xre.next(), xim.next()
                        k.op("pool", lambda e: e.tensor_tensor(out=xr[:], in0=a1[:], in1=a2[:], op=ALU.add), r=[a1, a2], w=[xr])
                        k.op("pool", lambda e: e.tensor_tensor(out=xi[:], in0=a3[:], in1=a4[:], op=ALU.subtract), r=[a3, a4], w=[xi])
                        st2.append((xr, xi))
                    st3 = []
                    for j in range(4):
                        xr, xi = st2[j]
                        gr, gi = gre.next(), gim.next()
                        k.op("dve", lambda e: e.tensor_tensor_scan(out=V(gr[:]), data0=Ar[j][:], data1=V(xr[:]),
                                                                   initial=car_re[j][:, 0:1], op0=ALU.mult, op1=ALU.add),
                             r=[Ar[j], xr, car_re[j]], w=[gr])
                        k.op("dve", lambda e: e.tensor_tensor_scan(out=V(gi[:]), data0=Ar[j][:], data1=V(xi[:]),
                                                                   initial=car_im[j][:, 0:1], op0=ALU.mult, op1=ALU.add),
                             r=[Ar[j], xi, car_im[j]], w=[gi])
                        st3.append((gr, gi))
                    st4 = []
                    for j in range(4):
                        gr, gi = st3[j]
                        cs, sn = st1[j][4], st1[j][5]
                        b1, b2, b3, b4 = t1.next(), t2.next(), t3.next(), t4.next()
                        k.op("pool", lambda e: e.tensor_tensor(out=b1[:], in0=gr[:], in1=cs, op=ALU.mult), r=[gr, cosT[j]], w=[b1])
                        k.op("pool", lambda e: e.tensor_tensor(out=b2[:], in0=gi[:], in1=sn, op=ALU.mult), r=[gi, sinT[j]], w=[b2])
                        k.op("pool", lambda e: e.tensor_tensor(out=b3[:], in0=gr[:], in1=sn, op=ALU.mult), r=[gr, sinT[j]], w=[b3])
                        k.op("pool", lambda e: e.tensor_tensor(out=b4[:], in0=gi[:], in1=cs, op=ALU.mult), r=[gi, cosT[j]], w=[b4])
                        st4.append((b1, b2, b3, b4))
                    for j in range(4):
                        b1, b2, b3, b4 = st4[j]
                        hr, hi = hre.next(), him.next()
                        k.op("dve", lambda e: e.tensor_tensor(out=hr[:], in0=b1[:], in1=b2[:], op=ALU.subtract), r=[b1, b2], w=[hr])
                        k.op("dve", lambda e: e.tensor_tensor(out=hi[:], in0=b3[:], in1=b4[:], op=ALU.add), r=[b3, b4], w=[hi])
                        last = 0 if rev else TB - 1
                        k.op("dve", lambda e: e.tensor_copy(out=car_re[j][:], in_=hr[:, last:last + 1]), r=[hr], w=[car_re[j]])
                        k.op("dve", lambda e: e.tensor_copy(out=car_im[j][:], in_=hi[:, last:last + 1]), r=[hi], w=[car_im[j]])
                        mm(k, yps[:], Cre[j][:], hr[:], j == 0, False, r=[Cre[j], hr], w=[yps])
                        mm(k, yps[:], Cim[j][:], hi[:], False, (j == 3) and rev, r=[Cim[j], hi], w=[yps])
                    ys = ysb.next()
                    if not rev:
                        mm(k, yps[:], dsk[:], ub[:], False, True, r=[dsk, ub], w=[yps])
                        k.op("act", lambda e: e.activation(out=ys[:], in_=yps[:], func=AF.Copy), r=[yps], w=[ys])
                    else:
                        yo = yold.next()
                        k.dma("sp", lambda e: e.dma_start(out=yo[:], in_=io["yT"][cc * 128:(cc + 1) * 128, b * TB:(b + 1) * TB]),
                              r=[io["d_yT"]], w=[yo])
                        k.op("dve", lambda e: e.tensor_tensor(out=ys[:], in0=yps[:], in1=yo[:], op=ALU.add), r=[yps, yo], w=[ys])
                    k.dma("sp", lambda e: e.dma_start(out=io["yT"][cc * 128:(cc + 1) * 128, b * TB:(b + 1) * TB], in_=ys[:]),
                          r=[ys], w=[io["d_yT"]])


def phase_even_out(k, cfg, io, layer):
    S = cfg.S
    ev = layer // 2
    h = io["out"]
    TS = min(512, S)
    HALO = 8
    with Phase(k, "a3") as P:
        c = make_consts(k, P)
        wglu = P.sb([128, 4, 512], BF16)
        load_w_bf16(k, wglu, io["s5_w_glu"][ev], 4)
        wout = P.sb([128, 8, D], BF16)
        load_w_bf16(k, wout, io["hyb_w_out"][ev], 8)
        pw = P.sb([128, 4, 128], BF16)
        for gi in range(4):
            k.dma("pool", lambda e: e.dma_start(out=pw[:, gi, :], in_=io["pool_w"][ev, gi]), w=[pw])
        bglu = P.sb([128, 4], F32)
        k.dma("sp", lambda e: e.dma_start(out=bglu[:], in_=io["s5_b_glu"][ev:ev + 1, :].rearrange("o (c p) -> p (o c)", p=128)),
              w=[bglu])
        psc = P.sb([128, 4], F32)
        k.dma("sp", lambda e: e.dma_start(out=psc[:], in_=io["pool_scale"][ev:ev + 1, :].rearrange("o (c p) -> p (o c)", p=128)),
              w=[psc])
        iot = P.sb([128, TS], F32)
        k.op("pool", lambda e: e.iota(iot[:], pattern=[[1, TS]], base=0, channel_multiplier=0,
                                      allow_small_or_imprecise_dtypes=True), w=[iot])
        yt_ring = Ring([P.sb([128, TS], F32) for _ in range(2)])
        yg = P.sb([128, 4, TS], F32)
        ygb = P.sb([128, 4, TS], BF16)
        catT = P.sb([128, 8, TS], BF16)
        glp = Ring([P.ps([128, TS], F32) for _ in range(2)])
        sg = Ring([P.sb([128, TS], F32) for _ in range(2)])
        W = TS + 2 * HALO
        zv = Ring([P.sb([128, W], F32) for _ in range(2)])
        sA = P.sb([128, W], F32)
        sB = P.sb([128, W], F32)
        hi_t = P.sb([128, TS], F32)
        lo_t = P.sb([128, TS], F32)
        pT = Ring([P.sb([128, TS], BF16) for _ in range(2)])
        mxp = Ring([P.ps([128, 512], F32) for _ in range(2)])
        ht_ring = Ring([P.sb([128, D], F32) for _ in range(2)])
        for t0 in range(0, S, TS):
            for cc in range(4):
                yt = yt_ring.next()
                k.dma("sp", lambda e: e.dma_start(out=yt[:], in_=io["yT"][cc * 128:(cc + 1) * 128, t0:t0 + TS]),
                      r=[io["d_yT"]], w=[yt])
                k.op("act", lambda e: e.activation(out=yg[:, cc, :], in_=yt[:], func=AF.Gelu), r=[yt], w=[yg])
            k.op("dve", lambda e: e.tensor_copy(out=ygb[:], in_=yg[:]), r=[yg], w=[ygb])
            for c2 in range(4):
                gp = glp.next()
                for cc in range(4):
                    mm(k, gp[:], wglu[:, cc, c2 * 128:(c2 + 1) * 128], ygb[:, cc, :], cc == 0, cc == 3, r=[wglu, ygb], w=[gp])
                s = sg.next()
                k.op("act", lambda e: e.activation(out=s[:], in_=gp[:], func=AF.Sigmoid, bias=bglu[:, c2:c2 + 1]),
                     r=[gp, bglu], w=[s])
                k.op("dve", lambda e: e.tensor_tensor(out=catT[:, c2, :], in0=yg[:, c2, :], in1=s[:], op=ALU.mult),
                     r=[yg, s], w=[catT])
            for gi, win in enumerate((2, 4, 8, 16)):
                z = zv.next()
                lo = max(t0 - HALO, 0)
                hi = min(t0 + TS + HALO, S)
                k.op("pool", lambda e: e.memset(z[:], 0.0), w=[z])
                k.dma("sp", lambda e: e.dma_start(out=z[:, lo - (t0 - HALO):hi - (t0 - HALO)],
                                                  in_=io["zpT"][gi * 128:(gi + 1) * 128, lo:hi]), r=[io["d_zpT"]], w=[z])
                k.op("dve", lambda e: e.memset(sA[:], 0.0), w=[sA])
                k.op("dve", lambda e: e.tensor_tensor(out=sA[:, 1:W], in0=z[:, 0:W - 1], in1=z[:, 1:W], op=ALU.add), r=[z], w=[sA])
                cur, oth = sA, sB
                half = 1
                while 2 * half < win:
                    sh = half
                    k.op("dve", lambda e: e.memset(oth[:], 0.0), w=[oth])
                    k.op("dve", lambda e: e.tensor_tensor(out=oth[:, sh:W - sh], in0=cur[:, 0:W - 2 * sh], in1=cur[:, 2 * sh:W],
                                                          op=ALU.add), r=[cur], w=[oth])
                    cur, oth = oth, cur
                    half *= 2
                k.op("dve", lambda e: e.tensor_scalar(out=hi_t[:], in0=iot[:], scalar1=float(t0 + win // 2), scalar2=float(S),
                                                      op0=ALU.add, op1=ALU.min), r=[iot], w=[hi_t])
                k.op("dve", lambda e: e.tensor_scalar(out=lo_t[:], in0=iot[:], scalar1=float(t0 - win // 2), scalar2=0.0,
                                                      op0=ALU.add, op1=ALU.max), r=[iot], w=[lo_t])
                k.op("dve", lambda e: e.tensor_tensor(out=hi_t[:], in0=hi_t[:], in1=lo_t[:], op=ALU.subtract),
                     r=[hi_t, lo_t], w=[hi_t])
                k.op("dve", lambda e: e.reciprocal(out=hi_t[:], in_=hi_t[:]), r=[hi_t], w=[hi_t])
                k.op("dve", lambda e: e.tensor_tensor(out=lo_t[:], in0=cur[:, HALO:HALO + TS], in1=hi_t[:], op=ALU.mult),
                     r=[cur, hi_t], w=[lo_t])
                p_ = pT.next()
                k.op("dve", lambda e: e.tensor_tensor(out=p_[:], in0=lo_t[:], in1=z[:, HALO:HALO + TS], op=ALU.subtract),
                     r=[lo_t, z], w=[p_])
                gp = glp.next()
                mm(k, gp[:], pw[:, gi, :], p_[:], True, True, r=[pw, p_], w=[gp])
                k.op("act", lambda e: e.activation(out=catT[:, 4 + gi, :], in_=gp[:], func=AF.Copy, scale=psc[:, gi:gi + 1]),
                     r=[gp, psc], w=[catT])
            for st in range(TS // 128):
                ht = ht_ring.next()
                r0 = t0 + st * 128
                k.dma("sp", lambda e: e.dma_start(out=ht[:], in_=h[r0:r0 + 128, :]), r=[io["d_h"]], w=[ht])
                for hf in range(2):
                    mp = mxp.next()
                    for c8 in range(8):
                        mm(k, mp[:], catT[:, c8, st * 128:(st + 1) * 128], wout[:, c8, hf * 512:(hf + 1) * 512],
                           c8 == 0, c8 == 7, r=[catT, wout], w=[mp])
                    k.op("dve", lambda e: e.tensor_tensor(out=ht[:, hf * 512:(hf + 1) * 512], in0=mp[:],
                                                          in1=ht[:, hf * 512:(hf + 1) * 512], op=ALU.add), r=[mp, ht], w=[ht])
                k.dma("sp", lambda e: e.dma_start(out=h[r0:r0 + 128, :], in_=ht[:]), r=[ht], w=[io["d_h"]])


def phase_moe(k, cfg, io, layer):
    S, E, FF, CAP = cfg.S, cfg.E, cfg.FF, cfg.CAP
    h = io["out"]
    FC = FF // 128
    NJT = CAP // 128
    with Phase(k, "m1") as P:
        c = make_consts(k, P)
        gbc = P.sb([128, D], F32)
        bcast_row(k, gbc, io["norm_ffn_g"][layer:layer + 1, :])
        wr = P.sb([128, DC, E], F32)
        k.dma("sp", lambda e: e.dma_start(out=wr[:], in_=io["moe_w_router"][layer].rearrange("(c p) e -> p c e", p=128)), w=[wr])
        ring_h = Ring([P.sb([128, D], F32) for _ in range(2)])
        junk = P.sb([128, D], F32)
        ss = P.sb([128, 1], F32)
        rstd = P.sb([128, 1], F32)
        n32 = Ring([P.sb([128, D], F32) for _ in range(2)])
        nbf = Ring([P.sb([128, D], BF16) for _ in range(2)])
        tp32 = Ring([P.ps([128, D], F32) for _ in range(2)])
        nT32 = Ring([P.sb([128, D], F32) for _ in range(2)])
        lgp = Ring([P.ps([128, E], F32) for _ in range(2)])
        mx = P.sb([128, 1], F32)
        se = P.sb([128, 1], F32)
        ex = P.sb([128, E], F32)
        aff = Ring([P.sb([128, E], F32) for _ in range(2)])
        atp = Ring([P.ps([E, 128], F32) for _ in range(2)])
        affT = P.sb([E, S], F32)
        for t0 in range(0, S, 128):
            ht = ring_h.next()
            k.dma("sp", lambda e: e.dma_start(out=ht[:], in_=h[t0:t0 + 128, :]), r=[io["d_h"]], w=[ht])
            rms_rstd(k, ht[:], ht, junk, ss, rstd, D)
            n = n32.next()
            k.op("dve", lambda e: e.scalar_tensor_tensor(out=n[:], in0=ht[:], scalar=rstd[:, 0:1], in1=gbc[:],
                                                         op0=ALU.mult, op1=ALU.mult), r=[ht, rstd, gbc], w=[n])
            nb = nbf.next()
            k.op("act", lambda e: e.activation(out=nb[:], in_=n[:], func=AF.Copy), r=[n], w=[nb])
            k.dma("sp", lambda e: e.dma_start(out=io["hn"][t0:t0 + 128, :], in_=nb[:]), r=[nb], w=[io["d_hn"]])
            tp = tp32.next()
            for dc in range(DC):
                tr(k, tp[:, dc * 128:(dc + 1) * 128], n[:, dc * 128:(dc + 1) * 128], c["idf"][:], r=[n, c["idf"]], w=[tp])
            nt = nT32.next()
            k.op("dve", lambda e: e.tensor_copy(out=nt[:], in_=tp[:]), r=[tp], w=[nt])
            lg = lgp.next()
            for dc in range(DC):
                mm(k, lg[:], nt[:, dc * 128:(dc + 1) * 128], wr[:, dc, :], dc == 0, dc == DC - 1, r=[nt, wr], w=[lg])
            k.op("dve", lambda e: e.tensor_reduce(out=mx[:], in_=lg[:], op=ALU.max, axis=AX.X), r=[lg], w=[mx])
            k.op("dve", lambda e: e.tensor_scalar(out=mx[:], in0=mx[:], scalar1=-1.0, scalar2=None, op0=ALU.mult), r=[mx], w=[mx])
            k.op("act", lambda e: e.activation(out=ex[:], in_=lg[:], func=AF.Exp, bias=mx[:, 0:1], accum_out=se[:, 0:1]),
                 r=[lg, mx], w=[ex, se])
            k.op("dve", lambda e: e.reciprocal(out=se[:], in_=se[:]), r=[se], w=[se])
            a = aff.next()
            k.op("dve", lambda e: e.tensor_scalar(out=a[:], in0=ex[:], scalar1=se[:, 0:1], scalar2=None, op0=ALU.mult),
                 r=[ex, se], w=[a])
            k.dma("sp", lambda e: e.dma_start(out=io["affd"][t0:t0 + 128, :], in_=a[:]), r=[a], w=[io["d_affd"]])
            ap_ = atp.next()
            tr(k, ap_[:], a[:], c["idf"][:], r=[a, c["idf"]], w=[ap_])
            k.op("dve", lambda e: e.tensor_copy(out=affT[:, t0:t0 + 128], in_=ap_[:]), r=[ap_], w=[affT])
        k.dma("sp", lambda e: e.dma_start(out=io["csd"][:, :], in_=affT[:]), r=[affT], w=[io["d_csd"]])

    with Phase(k, "m2") as P:
        affT = P.sb([E, S], F32)
        k.dma("sp", lambda e: e.dma_start(out=affT[:], in_=io["csd"][:, :]), r=[io["d_csd"]], w=[affT])
        msk = P.sb([E, S], BF16)
        lo = P.sb([E, 1], F32)
        hi = P.sb([E, 1], F32)
        mid = P.sb([E, 1], F32)
        cnt = P.sb([E, 1], F32)
        sel = P.sb([E, 1], U8)
        nsel = P.sb([E, 1], U8)
        k.op("dve", lambda e: e.memset(lo[:], 0.0), w=[lo])
        k.op("dve", lambda e: e.memset(hi[:], 1.0), w=[hi])
        for it in range(40):
            k.op("dve", lambda e: e.tensor_tensor(out=mid[:], in0=lo[:], in1=hi[:], op=ALU.add), r=[lo, hi], w=[mid])
            k.op("dve", lambda e: e.tensor_scalar(out=mid[:], in0=mid[:], scalar1=0.5, scalar2=None, op0=ALU.mult), r=[mid], w=[mid])
            k.op("dve", lambda e: e.tensor_scalar(out=msk[:], in0=affT[:], scalar1=mid[:, 0:1], scalar2=0.0, op0=ALU.is_gt,
                                                  op1=ALU.add, accum_out=cnt[:, 0:1]), r=[affT, mid], w=[msk, cnt])
            k.op("dve", lambda e: e.tensor_scalar(out=sel[:], in0=cnt[:], scalar1=float(CAP), scalar2=None, op0=ALU.is_ge),
                 r=[cnt], w=[sel])
            k.op("dve", lambda e: e.tensor_scalar(out=nsel[:], in0=cnt[:], scalar1=float(CAP), scalar2=None, op0=ALU.is_lt),
                 r=[cnt], w=[nsel])
            k.op("dve", lambda e: e.copy_predicated(out=lo[:], mask=sel[:], data=mid[:]), r=[sel, mid], w=[lo])
            k.op("dve", lambda e: e.copy_predicated(out=hi[:], mask=nsel[:], data=mid[:]), r=[nsel, mid], w=[hi])
        k.op("dve", lambda e: e.tensor_scalar(out=msk[:], in0=affT[:], scalar1=lo[:, 0:1], scalar2=None, op0=ALU.is_gt),
             r=[affT, lo], w=[msk])
        k.op("dve", lambda e: e.tensor_tensor_scan(out=affT[:], data0=msk[:], data1=msk[:], initial=0.0,
                                                   op0=ALU.add, op1=ALU.max), r=[msk], w=[affT])
        k.dma("sp", lambda e: e.dma_start(out=io["csd"][:, :], in_=affT[:]), r=[affT], w=[io["d_csd"]])
        NT = S // 128
        ccs = P.sb([E, NT], F32)
        k.op("dve", lambda e: e.tensor_copy(out=ccs[:], in_=affT[:, 127::128]), r=[affT], w=[ccs])
        k.dma("sp", lambda e: e.dma_start(out=io["ccd"][:, :], in_=ccs[:]), r=[ccs], w=[io["d_ccd"]])
    with Phase(k, "m3") as P:
        NT = S // 128
        ccb = Ring([P.sb([128, NT], F32) for _ in range(2)])
        junk = P.sb([128, 128], F32)
        jall = P.sb([128, NJT], F32)
        k.op("pool", lambda e: e.iota(jall[:], pattern=[[128, NJT]], base=0, channel_multiplier=1,
                                      allow_small_or_imprecise_dtypes=True), w=[jall])
        mj = Ring([P.sb([128, 1], F32) for _ in range(3)])
        rowf = Ring([P.sb([128, 1], F32) for _ in range(3)])
        rowi = Ring([P.sb([128, 1], I32) for _ in range(3)])
        cf = Ring([P.sb([128, 128], F32) for _ in range(3)])
        fine = Ring([P.sb([128, 1], F32) for _ in range(3)])
        idxf = Ring([P.sb([128, 1], F32) for _ in range(3)])
        idx_i = P.sb([128, E * NJT], I32)
        csrows = io["csd"].rearrange("e (m t) -> (e m) t", t=128)
        for ex_ in range(E):
            cb_ = ccb.next()
            k.dma("sp", lambda e: e.dma_start(out=cb_[:], in_=io["ccd"][ex_:ex_ + 1, :].to_broadcast([128, NT])),
                  r=[io["d_ccd"]], w=[cb_])
            for jt in range(NJT):
                col = ex_ * NJT + jt
                m_ = mj.next()
                k.op("dve", lambda e: e.tensor_scalar(out=junk[:, 0:NT], in0=cb_[:], scalar1=jall[:, jt:jt + 1], scalar2=0.0,
                                                      op0=ALU.is_le, op1=ALU.add, accum_out=m_[:, 0:1]),
                     r=[cb_, jall], w=[junk, m_])
                k.op("dve", lambda e: e.tensor_scalar(out=m_[:], in0=m_[:], scalar1=float(NT - 1), scalar2=None, op0=ALU.min),
                     r=[m_], w=[m_])
                rf = rowf.next()
                k.op("dve", lambda e: e.tensor_scalar(out=rf[:], in0=m_[:], scalar1=float(ex_ * NT), scalar2=None, op0=ALU.add),
                     r=[m_], w=[rf])
                ri = rowi.next()
                k.op("dve", lambda e: e.tensor_copy(out=ri[:], in_=rf[:]), r=[rf], w=[ri])
                cf_ = cf.next()
                k.dma("pool", lambda e: e.indirect_dma_start(
                    out=cf_[:], out_offset=None, in_=csrows,
                    in_offset=bass.IndirectOffsetOnAxis(ap=ri[:, 0:1], axis=0)), r=[ri, io["d_csd"]], w=[cf_])
                fn_ = fine.next()
                k.op("dve", lambda e: e.tensor_scalar(out=junk[:], in0=cf_[:], scalar1=jall[:, jt:jt + 1], scalar2=0.0,
                                                      op0=ALU.is_le, op1=ALU.add, accum_out=fn_[:, 0:1]),
                     r=[cf_, jall], w=[junk, fn_])
                xf = idxf.next()
                k.op("dve", lambda e: e.scalar_tensor_tensor(out=xf[:], in0=m_[:], scalar=128.0, in1=fn_[:],
                                                             op0=ALU.mult, op1=ALU.add), r=[m_, fn_], w=[xf])
                k.op("dve", lambda e: e.tensor_scalar(out=xf[:], in0=xf[:], scalar1=float(S - 1), scalar2=None, op0=ALU.min),
                     r=[xf], w=[xf])
                k.op("dve", lambda e: e.tensor_copy(out=idx_i[:, col:col + 1], in_=xf[:]), r=[xf], w=[idx_i])
        k.dma("sp", lambda e: e.dma_start(out=io["idxd"][:, :], in_=idx_i[:]), r=[idx_i], w=[io["d_idxd"]])

    with Phase(k, "m4") as P:
        c = make_consts(k, P)
        idx_i = P.sb([128, E * NJT], I32)
        k.dma("sp", lambda e: e.dma_start(out=idx_i[:], in_=io["idxd"][:, :]), r=[io["d_idxd"]], w=[idx_i])
        wg = P.sb([128, DC, FF], BF16)
        wu = P.sb([128, DC, FF], BF16)
        wd = P.sb([128, FC, D], BF16)
        GS = min(4, NJT)
        NG = NJT // GS
        xg = Ring([P.sb([128, D], BF16) for _ in range(3)])
        ar = Ring([P.sb([128, E], F32) for _ in range(2 * GS)])
        tpx = Ring([P.ps([128, D], BF16) for _ in range(2)])
        xgT = P.sb([128, DC, GS * 128], BF16)
        actT = P.sb([128, FC, GS * 128], BF16)
        pa = Ring([P.ps([128, GS * 128], F32) for _ in range(2)])
        pu = Ring([P.ps([128, GS * 128], F32) for _ in range(2)])
        sa = Ring([P.sb([128, GS * 128], F32) for _ in range(2)])
        py = Ring([P.ps([128, 512], F32) for _ in range(2)])
        ys = Ring([P.sb([128, D], F32) for _ in range(2)])
        for ex_ in range(E):
            load_w_bf16(k, wg, io["moe_w_gate"][layer, ex_], DC)
            load_w_bf16(k, wu, io["moe_w_up"][layer, ex_], DC)
            load_w_bf16(k, wd, io["moe_w_down"][layer, ex_], FC)
            for g in range(NG):
                gates = []
                for st in range(GS):
                    col = ex_ * NJT + g * GS + st
                    x_ = xg.next()
                    k.dma("pool", lambda e: e.indirect_dma_start(
                        out=x_[:], out_offset=None, in_=io["hn"],
                        in_offset=bass.IndirectOffsetOnAxis(ap=idx_i[:, col:col + 1], axis=0)),
                        r=[idx_i, io["d_hn"]], w=[x_])
                    a_ = ar.next()
                    k.dma("pool", lambda e: e.indirect_dma_start(
                        out=a_[:], out_offset=None, in_=io["affd"],
                        in_offset=bass.IndirectOffsetOnAxis(ap=idx_i[:, col:col + 1], axis=0)),
                        r=[idx_i, io["d_affd"]], w=[a_])
                    gates.append(a_)
                    tp = tpx.next()
                    for dc in range(DC):
                        tr(k, tp[:, dc * 128:(dc + 1) * 128], x_[:, dc * 128:(dc + 1) * 128], c["idb"][:], r=[x_, c["idb"]], w=[tp])
                    k.op("act", lambda e: e.activation(out=xgT[:, :, st * 128:(st + 1) * 128],
                                                       in_=tp[:].rearrange("p (c t) -> p c t", c=DC), func=AF.Copy),
                         r=[tp], w=[xgT])
                for fc in range(FC):
                    a_p, u_p = pa.next(), pu.next()
                    for dc in range(DC):
                        mm(k, a_p[:], wg[:, dc, fc * 128:(fc + 1) * 128], xgT[:, dc, :], dc == 0, dc == DC - 1, r=[wg, xgT], w=[a_p])
                    for dc in range(DC):
                        mm(k, u_p[:], wu[:, dc, fc * 128:(fc + 1) * 128], xgT[:, dc, :], dc == 0, dc == DC - 1, r=[wu, xgT], w=[u_p])
                    s_ = sa.next()
                    k.op("act", lambda e: e.activation(out=s_[:], in_=a_p[:], func=AF.Silu), r=[a_p], w=[s_])
                    k.op("dve", lambda e: e.tensor_tensor(out=actT[:, fc, :], in0=u_p[:], in1=s_[:], op=ALU.mult),
                         r=[u_p, s_], w=[actT])
                for st in range(GS):
                    col = ex_ * NJT + g * GS + st
                    y_ = ys.next()
                    for hf in range(2):
                        yp = py.next()
                        for fc in range(FC):
                            mm(k, yp[:], actT[:, fc, st * 128:(st + 1) * 128], wd[:, fc, hf * 512:(hf + 1) * 512],
                               fc == 0, fc == FC - 1, r=[actT, wd], w=[yp])
                        k.op("dve", lambda e: e.tensor_scalar(out=y_[:, hf * 512:(hf + 1) * 512], in0=yp[:],
                                                              scalar1=gates[st][:, ex_:ex_ + 1], scalar2=None, op0=ALU.mult),
                             r=[yp, gates[st]], w=[y_])
                    k.dma("pool", lambda e: e.indirect_dma_start(
                        out=h, out_offset=bass.IndirectOffsetOnAxis(ap=idx_i[:, col:col + 1], axis=0),
                        in_=y_[:], in_offset=None, compute_op=ALU.add), r=[y_, idx_i], w=[io["d_h"]])


def phase_attn(k, cfg, io, layer):
    S = cfg.S
    od = layer // 2
    h = io["out"]
    TS = min(512, S)
    NST = TS // 128
    lam_init = 0.8 - 0.6 * math.exp(-0.3 * layer)
    with Phase(k, "b1") as P:
        c = make_consts(k, P)
        gbc = P.sb([128, D], F32)
        bcast_row(k, gbc, io["norm_mix_g"][layer:layer + 1, :])
        wqkv = P.sb([128, DC, 3 * D], BF16)
        load_w_bf16(k, wqkv, io["attn_w_qkv"][od], DC)
        gq = P.sb([128, 64], F32)
        gk = P.sb([128, 64], F32)
        bcast_row(k, gq, io["attn_q_norm_g"][od:od + 1, :])
        bcast_row(k, gk, io["attn_k_norm_g"][od:od + 1, :])
        k.op("dve", lambda e: e.tensor_scalar(out=gq[:], in0=gq[:], scalar1=0.125, scalar2=None, op0=ALU.mult), r=[gq], w=[gq])
        inv = P.sb([128, 32], F32)
        k.op("pool", lambda e: e.iota(inv[:], pattern=[[1, 32]], base=0, channel_multiplier=0,
                                      allow_small_or_imprecise_dtypes=True), w=[inv])
        k.op("act", lambda e: e.activation(out=inv[:], in_=inv[:], func=AF.Exp, scale=-math.log(10000.0) / 32.0), r=[inv], w=[inv])
        ring_h = Ring([P.sb([128, D], F32) for _ in range(2)])
        junk = P.sb([128, D], F32)
        ss = P.sb([128, 1], F32)
        rstd = P.sb([128, 1], F32)
        nb_ring = Ring([P.sb([128, D], BF16) for _ in range(2)])
        tp_ring = Ring([P.ps([128, D], BF16) for _ in range(1)])
        nT = P.sb([128, DC, TS], BF16)
        qkvp = [P.ps([128, 512], F32) for _ in range(6)]
        posi = P.sb([128, 1], I32)
        posf = P.sb([128, 1], F32)
        sn = P.sb([128, 32], F32)
        cs = P.sb([128, 32], F32)
        tm = P.sb([128, 32], F32)
        m_ = P.sb([128, 32], F32)
        sq = P.sb([128, D], F32)
        ssq = P.sb([128, 16], F32)
        qn = P.sb([128, D], F32)
        ra = P.sb([128, 16, 32], F32)
        rb = P.sb([128, 16, 32], F32)
        qr = P.sb([128, D], BF16)
        tpq = P.ps([128, D], BF16)
        qTs = P.sb([128, 8, TS], BF16)
        kTs = P.sb([128, 8, TS], BF16)
        vb = Ring([P.sb([128, D], BF16) for _ in range(2)])
        for t0 in range(0, S, TS):
            norm_supertile(k, c, h, t0, NST, gbc, ring_h, junk, ss, rstd, nb_ring, tp_ring, nT)
            for st in range(NST):
                r0 = t0 + st * 128
                for j in range(6):
                    for dc in range(DC):
                        mm(k, qkvp[j][:], nT[:, dc, st * 128:(st + 1) * 128], wqkv[:, dc, j * 512:(j + 1) * 512],
                           dc == 0, dc == DC - 1, r=[nT, wqkv], w=[qkvp[j]])
                k.dma("sp", lambda e: e.dma_start(out=posi[:], in_=io["positions"][r0:r0 + 128, :]), w=[posi])
                k.op("dve", lambda e: e.tensor_copy(out=posf[:], in_=posi[:]), r=[posi], w=[posf])
                k.op("dve", lambda e: e.tensor_scalar(out=sn[:], in0=inv[:], scalar1=posf[:, 0:1], scalar2=None, op0=ALU.mult),
                     r=[inv, posf], w=[sn])
                k.op("dve", lambda e: e.tensor_copy(out=cs[:], in_=sn[:]), r=[sn], w=[cs])
                range_reduce(k, sn, tm, m_, 32, 0.0)
                range_reduce(k, cs, tm, m_, 32, math.pi / 2)
                k.op("act", lambda e: e.activation(out=sn[:], in_=sn[:], func=AF.Sin), r=[sn], w=[sn])
                k.op("act", lambda e: e.activation(out=cs[:], in_=cs[:], func=AF.Sin), r=[cs], w=[cs])
                csb = cs[:].unsqueeze(1).to_broadcast([128, 16, 32])
                snb = sn[:].unsqueeze(1).to_broadcast([128, 16, 32])
                for which, g_t, dstT in ((0, gq, qTs), (1, gk, kTs)):
                    pA, pB = qkvp[2 * which], qkvp[2 * which + 1]
                    for hf, pp_ in enumerate((pA, pB)):
                        k.op("act", lambda e: e.activation(out=sq[:, hf * 512:(hf + 1) * 512], in_=pp_[:], func=AF.Square),
                             r=[pp_], w=[sq])
                    k.op("dve", lambda e: e.tensor_reduce(out=ssq[:], in_=sq[:].rearrange("p (a d) -> p a d", d=64),
                                                          op=ALU.add, axis=AX.X), r=[sq], w=[ssq])
                    k.op("dve", lambda e: e.tensor_scalar(out=ssq[:], in0=ssq[:], scalar1=1.0 / 64, scalar2=EPS,
                                                          op0=ALU.mult, op1=ALU.add), r=[ssq], w=[ssq])
                    k.op("act", lambda e: e.activation(out=ssq[:], in_=ssq[:], func=AF.Sqrt), r=[ssq], w=[ssq])
                    k.op("dve", lambda e: e.reciprocal(out=ssq[:], in_=ssq[:]), r=[ssq], w=[ssq])
                    for hf, pp_ in enumerate((pA, pB)):
                        k.op("dve", lambda e: e.tensor_tensor(
                            out=qn[:, hf * 512:(hf + 1) * 512].rearrange("p (a d) -> p a d", d=64),
                            in0=pp_[:].rearrange("p (a d) -> p a d", d=64),
                            in1=ssq[:, hf * 8:(hf + 1) * 8].unsqueeze(2).to_broadcast([128, 8, 64]), op=ALU.mult),
                            r=[pp_, ssq], w=[qn])
                    k.op("dve", lambda e: e.tensor_tensor(out=qn[:].rearrange("p (a d) -> p a d", d=64),
                                                          in0=qn[:].rearrange("p (a d) -> p a d", d=64),
                                                          in1=g_t[:].unsqueeze(1).to_broadcast([128, 16, 64]), op=ALU.mult),
                         r=[qn, g_t], w=[qn])
                    q3 = qn[:].rearrange("p (a d) -> p a d", d=64)
                    o3 = qr[:].rearrange("p (a d) -> p a d", d=64)
                    x1, x2 = q3[:, :, 0:32], q3[:, :, 32:64]
                    k.op("dve", lambda e: e.tensor_tensor(out=ra[:], in0=x1, in1=csb, op=ALU.mult), r=[qn, cs], w=[ra])
                    k.op("dve", lambda e: e.tensor_tensor(out=rb[:], in0=x2, in1=snb, op=ALU.mult), r=[qn, sn], w=[rb])
                    k.op("dve", lambda e: e.tensor_tensor(out=o3[:, :, 0:32], in0=ra[:], in1=rb[:], op=ALU.subtract), r=[ra, rb], w=[qr])
                    k.op("dve", lambda e: e.tensor_tensor(out=ra[:], in0=x1, in1=snb, op=ALU.mult), r=[qn, sn], w=[ra])
                    k.op("dve", lambda e: e.tensor_tensor(out=rb[:], in0=x2, in1=csb, op=ALU.mult), r=[qn, cs], w=[rb])
                    k.op("dve", lambda e: e.tensor_tensor(out=o3[:, :, 32:64], in0=ra[:], in1=rb[:], op=ALU.add), r=[ra, rb], w=[qr])
                    for hh in range(8):
                        tr(k, tpq[:, hh * 128:(hh + 1) * 128], qr[:, hh * 128:(hh + 1) * 128], c["idb"][:], r=[qr, c["idb"]], w=[tpq])
                    k.op("act", lambda e: e.activation(out=dstT[:, :, st * 128:(st + 1) * 128],
                                                       in_=tpq[:].rearrange("p (a t) -> p a t", a=8), func=AF.Copy),
                         r=[tpq], w=[dstT])
                v_ = vb.next()
                for hf in range(2):
                    k.op("act", lambda e: e.activation(out=v_[:, hf * 512:(hf + 1) * 512], in_=qkvp[4 + hf][:], func=AF.Copy),
                         r=[qkvp[4 + hf]], w=[v_])
                k.dma("sp", lambda e: e.dma_start(out=io["Vd"][r0:r0 + 128, :], in_=v_[:]), r=[v_], w=[io["d_Vd"]])
            k.dma("sp", lambda e: e.dma_start(out=io["QT"][:, :, t0:t0 + TS].rearrange("a p t -> p a t"), in_=qTs[:]),
                  r=[qTs], w=[io["d_QT"]])
            k.dma("sp", lambda e: e.dma_start(out=io["KT"][:, :, t0:t0 + TS].rearrange("a p t -> p a t"), in_=kTs[:]),
                  r=[kTs], w=[io["d_KT"]])

    with Phase(k, "b2") as P:
        c = make_consts(k, P)
        lv = P.sb([1, 4, 64], F32)
        for i, nm in enumerate(("attn_lam_q1", "attn_lam_k1", "attn_lam_q2", "attn_lam_k2")):
            k.dma("sp", lambda e: e.dma_start(out=lv[:, i, :], in_=io[nm][od:od + 1, :]), w=[lv])
        pr2 = P.sb([1, 2, 64], F32)
        k.op("dve", lambda e: e.tensor_tensor(out=pr2[:, 0, :], in0=lv[:, 0, :], in1=lv[:, 1, :], op=ALU.mult), r=[lv], w=[pr2])
        k.op("dve", lambda e: e.tensor_tensor(out=pr2[:, 1, :], in0=lv[:, 2, :], in1=lv[:, 3, :], op=ALU.mult), r=[lv], w=[pr2])
        s2 = P.sb([1, 2], F32)
        k.op("dve", lambda e: e.tensor_reduce(out=s2[:], in_=pr2[:], op=ALU.add, axis=AX.X), r=[pr2], w=[s2])
        k.op("act", lambda e: e.activation(out=s2[:], in_=s2[:], func=AF.Exp), r=[s2], w=[s2])
        l1 = P.sb([1, 2], F32)
        k.op("dve", lambda e: e.tensor_tensor(out=l1[:, 0:1], in0=s2[:, 1:2], in1=s2[:, 0:1], op=ALU.subtract), r=[s2], w=[l1])
        k.op("dve", lambda e: e.tensor_scalar(out=l1[:, 0:1], in0=l1[:, 0:1], scalar1=-lam_init, scalar2=None, op0=ALU.add),
             r=[l1], w=[l1])
        QB = min(512, S)
        NQT = QB // 128
        accb = [P.ps([128, 512], F32) for _ in range(3)]
        stb = Ring([P.ps([128, 2, QB], F32) for _ in range(2)])
        lp = accb[0]
        mm(k, lp[:, 0:1], c["onef"][0:1, 0:128], l1[:, 0:1], True, True, r=[c["onef"], l1], w=[lp])
        nlam = P.sb([128, 1], F32)
        k.op("dve", lambda e: e.tensor_copy(out=nlam[:], in_=lp[:, 0:1]), r=[lp], w=[nlam])
        gs = P.sb([128, 128], F32)
        bcast_row(k, gs, io["attn_subln_g"][od:od + 1, :])
        k.op("dve", lambda e: e.tensor_scalar(out=gs[:], in0=gs[:], scalar1=1.0 - lam_init, scalar2=None, op0=ALU.mult), r=[gs], w=[gs])
        zb = P.sb([128, 128], BF16)
        k.op("dve", lambda e: e.memset(zb[:], 0.0), w=[zb])
        KTs = P.sb([128, S], BF16)
        QTs = P.sb([128, S], BF16)
        NKT = S // 128
        Vs = P.sb([128, NKT, 129], BF16)
        k.op("dve", lambda e: e.memset(Vs[:, :, 128:129], 1.0), w=[Vs])
        pT = Ring([P.sb([128, 2, QB], BF16) for _ in range(3)])
        rz = P.sb([128, 2], F32)
        tt = Ring([P.sb([128, 128], F32) for _ in range(2)])
        oo = Ring([P.sb([128, 128], F32) for _ in range(2)])
        ss = P.sb([128, 4], F32)
        junk = P.sb([128, 128], F32)
        att = Ring([P.sb([128, NQT, 128], BF16) for _ in range(2)])

        def acc(sub, qt):
            i = sub * NQT + qt
            return accb[i // 3], (i % 3) * 129

        for hh in range(8):
            k.dma("sp", lambda e: e.dma_start(out=KTs[:], in_=io["KT"][hh]), r=[io["d_KT"]], w=[KTs])
            k.dma("sp", lambda e: e.dma_start(out=QTs[:], in_=io["QT"][hh]), r=[io["d_QT"]], w=[QTs])
            k.dma("sp", lambda e: e.dma_start(out=Vs[:, :, 0:128],
                                              in_=io["Vd"][:, hh * 128:(hh + 1) * 128].rearrange("(t p) d -> p t d", p=128)),
                  r=[io["d_Vd"]], w=[Vs])
            for q0 in range(0, S, QB):
                for ab in accb:
                    mm(k, ab[:, 0:QB], zb[:], QTs[:, q0:q0 + QB], True, False, r=[zb, QTs], w=[ab])
                def s_stage(kt):
                    sb_ = stb.next()
                    for sub in range(2):
                        rows = slice(sub * 64, (sub + 1) * 64)
                        mm(k, sb_[:, sub, :], KTs[rows, kt * 128:(kt + 1) * 128], QTs[rows, q0:q0 + QB], True, True,
                           r=[KTs, QTs], w=[sb_])
                    p_ = pT.next()
                    k.op("act", lambda e: e.activation(out=p_[:], in_=sb_[:], func=AF.Exp), r=[sb_], w=[p_])
                    return p_

                pend = s_stage(0)
                for kt in range(NKT):
                    nxt = s_stage(kt + 1) if kt + 1 < NKT else None
                    p_ = pend
                    for sub in range(2):
                        for qt in range(NQT):
                            ab, o_ = acc(sub, qt)
                            k.op("pe", lambda e: e.matmul(ab[:, o_:o_ + 129], p_[:, sub, qt * 128:(qt + 1) * 128], Vs[:, kt, :],
                                                          start=False, stop=(kt == NKT - 1), skip_group_check=True),
                                 r=[p_, Vs], w=[ab])
                    pend = nxt
                a_ = att.next()
                for qt in range(NQT):
                    a0, o0 = acc(0, qt)
                    a1, o1_ = acc(1, qt)
                    k.op("dve", lambda e: e.reciprocal(out=rz[:, 0:1], in_=a0[:, o0 + 128:o0 + 129]), r=[a0], w=[rz])
                    k.op("dve", lambda e: e.reciprocal(out=rz[:, 1:2], in_=a1[:, o1_ + 128:o1_ + 129]), r=[a1], w=[rz])
                    k.op("dve", lambda e: e.tensor_tensor(out=rz[:, 1:2], in0=rz[:, 1:2], in1=nlam[:, 0:1], op=ALU.mult),
                         r=[rz, nlam], w=[rz])
                    t_ = tt.next()
                    k.op("dve", lambda e: e.tensor_scalar(out=t_[:], in0=a0[:, o0:o0 + 128], scalar1=rz[:, 0:1], scalar2=None,
                                                          op0=ALU.mult), r=[a0, rz], w=[t_])
                    o_t = oo.next()
                    k.op("dve", lambda e: e.scalar_tensor_tensor(out=o_t[:], in0=a1[:, o1_:o1_ + 128], scalar=rz[:, 1:2], in1=t_[:],
                                                                 op0=ALU.mult, op1=ALU.add), r=[a1, rz, t_], w=[o_t])
                    k.op("act", lambda e: e.activation(out=junk[:], in_=o_t[:], func=AF.Square, accum_out=ss[:, qt:qt + 1]),
                         r=[o_t], w=[junk, ss])
                    k.op("dve", lambda e: e.tensor_scalar(out=ss[:, qt:qt + 1], in0=ss[:, qt:qt + 1], scalar1=1.0 / 128, scalar2=EPS,
                                                          op0=ALU.mult, op1=ALU.add), r=[ss], w=[ss])
                    k.op("act", lambda e: e.activation(out=ss[:, qt:qt + 1], in_=ss[:, qt:qt + 1], func=AF.Sqrt), r=[ss], w=[ss])
                    k.op("dve", lambda e: e.reciprocal(out=ss[:, qt:qt + 1], in_=ss[:, qt:qt + 1]), r=[ss], w=[ss])
                    k.op("dve", lambda e: e.scalar_tensor_tensor(out=a_[:, qt, :], in0=o_t[:], scalar=ss[:, qt:qt + 1], in1=gs[:],
                                                                 op0=ALU.mult, op1=ALU.mult), r=[o_t, ss, gs], w=[a_])
                k.dma("sp", lambda e: e.dma_start(
                    out=io["AO"][q0:q0 + QB, hh * 128:(hh + 1) * 128].rearrange("(t p) d -> p t d", p=128), in_=a_[:]),
                    r=[a_], w=[io["d_AO"]])

    with Phase(k, "b3") as P:
        c = make_consts(k, P)
        wo = P.sb([128, DC, D], BF16)
        load_w_bf16(k, wo, io["attn_w_out"][od], DC)
        ao = Ring([P.sb([128, D], BF16) for _ in range(2)])
        tp = Ring([P.ps([128, D], BF16) for _ in range(2)])
        aT = Ring([P.sb([128, DC, 128], BF16) for _ in range(2)])
        mp_r = Ring([P.ps([128, 512], F32) for _ in range(2)])
        ht_r = Ring([P.sb([128, D], F32) for _ in range(2)])
        for t0 in range(0, S, 128):
            a_ = ao.next()
            k.dma("sp", lambda e: e.dma_start(out=a_[:], in_=io["AO"][t0:t0 + 128, :]), r=[io["d_AO"]], w=[a_])
            t_ = tp.next()
            for dc in range(DC):
                tr(k, t_[:, dc * 128:(dc + 1) * 128], a_[:, dc * 128:(dc + 1) * 128], c["idb"][:], r=[a_, c["idb"]], w=[t_])
            at = aT.next()
            k.op("act", lambda e: e.activation(out=at[:], in_=t_[:].rearrange("p (c t) -> p c t", c=DC), func=AF.Copy), r=[t_], w=[at])
            ht = ht_r.next()
            k.dma("sp", lambda e: e.dma_start(out=ht[:], in_=h[t0:t0 + 128, :]), r=[io["d_h"]], w=[ht])
            for hf in range(2):
                mp = mp_r.next()
                for dc in range(DC):
                    mm(k, mp[:], at[:, dc, :], wo[:, dc, hf * 512:(hf + 1) * 512], dc == 0, dc == DC - 1, r=[at, wo], w=[mp])
                k.op("dve", lambda e: e.tensor_tensor(out=ht[:, hf * 512:(hf + 1) * 512], in0=mp[:],
                                                      in1=ht[:, hf * 512:(hf + 1) * 512], op=ALU.add), r=[mp, ht], w=[ht])
            k.dma("sp", lambda e: e.dma_start(out=h[t0:t0 + 128, :], in_=ht[:]), r=[ht], w=[io["d_h"]])


INPUT_SHAPES = None


def input_shapes(cfg):
    S, E, FF, DEPTH, NE, NO = cfg.S, cfg.E, cfg.FF, cfg.DEPTH, cfg.NE, cfg.NO
    sh = {
        "x": ([S, D], F32), "positions": ([S, 1], I32),
        "norm_mix_g": ([DEPTH, D], F32), "norm_ffn_g": ([DEPTH, D], F32),
        "hyb_w_in": ([NE, D, D], F32), "hyb_w_out": ([NE, D, D], F32),
        "s5_lam_re": ([NE, 2, 32, 64], F32), "s5_lam_im": ([NE, 2, 32, 64], F32), "s5_log_dt": ([NE, 2, 32], F32),
        "s5_b_re": ([NE, 2, 32, 64, 16], F32), "s5_b_im": ([NE, 2, 32, 64, 16], F32),
        "s5_c_re": ([NE, 2, 32, 16, 64], F32), "s5_c_im": ([NE, 2, 32, 16, 64], F32),
        "s5_d": ([NE, 512], F32), "s5_w_glu": ([NE, 512, 512], F32), "s5_b_glu": ([NE, 512], F32),
        "pool_w": ([NE, 4, 128, 128], F32), "pool_scale": ([NE, 512], F32),
        "moe_w_router": ([DEPTH, D, E], F32), "moe_w_gate": ([DEPTH, E, D, FF], F32),
        "moe_w_up": ([DEPTH, E, D, FF], F32), "moe_w_down": ([DEPTH, E, FF, D], F32),
    }
    if NO > 0:
        sh.update({
            "attn_w_qkv": ([NO, D, 3 * D], F32), "attn_w_out": ([NO, D, D], F32),
            "attn_q_norm_g": ([NO, 64], F32), "attn_k_norm_g": ([NO, 64], F32),
            "attn_lam_q1": ([NO, 64], F32), "attn_lam_k1": ([NO, 64], F32),
            "attn_lam_q2": ([NO, 64], F32), "attn_lam_k2": ([NO, 64], F32), "attn_subln_g": ([NO, 128], F32),
        })
    return sh


def build(cfg, phases=None):
    k = KB()
    nc = k.nc
    io = {}
    for nm, (shape, dt) in input_shapes(cfg).items():
        io[nm] = nc.dram_tensor(nm, shape, dt, kind="ExternalInput").ap()
    S, E = cfg.S, cfg.E
    io["out"] = nc.dram_tensor("out", [S, D], F32, kind="ExternalOutput").ap()
    scratch = {"uT": ([512, S], BF16), "zpT": ([512, S], F32), "yT": ([512, S], F32), "hn": ([S, D], BF16),
               "affd": ([S, E], F32), "csd": ([E, S], F32), "ccd": ([E, S // 128], F32), "idxd": ([128, E * (cfg.CAP // 128)], I32),
               "QT": ([8, 128, S], BF16), "KT": ([8, 128, S], BF16), "Vd": ([S, D], BF16), "AO": ([S, D], BF16)}
    for nm, (shape, dt) in scratch.items():
        io[nm] = nc.dram_tensor(nm, shape, dt, kind="Internal").ap()
        io["d_" + nm] = Dep()
    io["d_h"] = Dep()
    with nc.allow_non_contiguous_dma(reason="small strided parameter loads"):
        _emit(k, cfg, io, phases)
    k.barrier()
    return k


def _emit(k, cfg, io, phases):
    phase_copy_in(k, cfg, io["x"], io["out"])
    for layer in range(cfg.DEPTH):
        if layer % 2 == 0:
            if phases is None or "mix" in phases:
                phase_even_in(k, cfg, io, layer)
                phase_s5(k, cfg, io, layer)
                phase_even_out(k, cfg, io, layer)
        else:
            if phases is None or "mix" in phases:
                phase_attn(k, cfg, io, layer)
        if phases is None or "moe" in phases:
            phase_moe(k, cfg, io, layer)


_CACHE = {}


def kernel(**inputs):
    cfg = Cfg()
    if "k" not in _CACHE:
        _CACHE["k"] = build(cfg)
    k = _CACHE["k"]
    B = inputs["x"].shape[0]
    names = list(input_shapes(cfg).keys())
    in_maps = []
    for b in range(B):
        m = {}
        for nm in names:
            a = np.asarray(inputs[nm])
            if nm == "x":
                a = np.ascontiguousarray(a[b])
            elif nm == "positions":
                a = np.ascontiguousarray(a[b].reshape(cfg.S, 1).astype(np.int32))
            m[nm] = a
        in_maps.append(m)
    res = run_bass_kernel_spmd(k.nc, in_maps, core_ids=list(range(B)))
    return np.stack([np.asarray(r["out"]) for r in res.results], axis=0).astype(np.float32)
```
